# Optimizing a Trainium2 kernel written in Bass

```python
import jax
import jax.numpy as jnp
from jax import lax
import numpy as np

D_MODEL = 1024
BATCH = 8
SEQ = 4096
DEPTH = 2

CTX_LEN = 256
GRID_W = 64
HEAD_DIM = 64
NA_WIDTH = D_MODEL // 2
NA_HEADS = NA_WIDTH // HEAD_DIM
NA_WIN_ROWS = 8
NA_WIN_COLS = 16
NA_QCB = 16
NA_KCB = 32
RW_WIDTH = D_MODEL // 4
RW_HEADS = RW_WIDTH // HEAD_DIM
RW_DECAY_RANK = 64
RW_A_RANK = 64
RW_GATE_RANK = 128
RW_LORA = RW_DECAY_RANK + RW_A_RANK + RW_GATE_RANK
RW_IN = 3 * RW_WIDTH + 2 * RW_LORA
RW_GN_EPS = 64e-5
CV_WIDTH = D_MODEL // 4
CV_CONV_LEN = 31
MIX_WIDTH = NA_WIDTH + RW_WIDTH + CV_WIDTH
IN_WIDTH = 3 * NA_WIDTH + RW_IN + 2 * CV_WIDTH
D_FF = 2816
N_EXPERTS = 8
TOP_K = 2
D_FF_EXPERT = 3584
MOE_BLOCK = 128
N_DENSE_LAYERS = (DEPTH + 1) // 2
N_MOE_LAYERS = DEPTH // 2
RMS_EPS = 1e-6
LN_EPS = 1e-5

kernel_name = 'hybrid_natten_rwkv7_conformer_moe_dit'


def _rmsnorm(x, g):
    xf = x.astype(jnp.float32)
    y = xf * lax.rsqrt(jnp.mean(xf * xf, axis=-1, keepdims=True) + RMS_EPS)
    return (y * g.astype(jnp.float32)).astype(x.dtype)


def _layernorm(x, g, b):
    xf = x.astype(jnp.float32)
    mu = jnp.mean(xf, axis=-1, keepdims=True)
    var = jnp.mean(jnp.square(xf - mu), axis=-1, keepdims=True)
    y = (xf - mu) * lax.rsqrt(var + LN_EPS)
    return (y * g.astype(jnp.float32) + b.astype(jnp.float32)).astype(x.dtype)


def _modulate(h, shift, scale):
    return h * (1 + scale) + shift


def _swiglu(h, w_gate, w_up, w_down):
    return (jax.nn.silu(h @ w_gate) * (h @ w_up)) @ w_down


def _moe_swiglu(h, router, w_gate, w_up, w_down):
    shp = h.shape
    d = shp[-1]
    xt = h.reshape(-1, d)
    n_tok = xt.shape[0]
    n_asg = n_tok * TOP_K
    logits = (xt @ router).astype(jnp.float32)
    top_logit, top_e = lax.top_k(logits, TOP_K)
    gates = jax.nn.softmax(top_logit, axis=-1)
    flat_e = top_e.reshape(-1)
    flat_tok = jnp.arange(n_asg, dtype=jnp.int32) // TOP_K
    order = jnp.argsort(flat_e)
    s_e = flat_e[order]
    s_tok = flat_tok[order]
    s_gate = gates.reshape(-1)[order]
    counts = jnp.bincount(flat_e, length=N_EXPERTS)
    padded = (counts + MOE_BLOCK - 1) // MOE_BLOCK * MOE_BLOCK
    pad_end = jnp.cumsum(padded)
    pad_start = pad_end - padded
    start = jnp.cumsum(counts) - counts
    dest = pad_start[s_e] + jnp.arange(n_asg, dtype=jnp.int32) - start[s_e]
    n_blocks = (n_asg + MOE_BLOCK - 1) // MOE_BLOCK + N_EXPERTS
    slot_tok = jnp.full((n_blocks * MOE_BLOCK,), n_tok, jnp.int32).at[dest].set(s_tok)
    block_e = jnp.minimum(jnp.searchsorted(pad_end, jnp.arange(n_blocks) * MOE_BLOCK, side='right'), N_EXPERTS - 1)
    x_pad = jnp.concatenate([xt, jnp.zeros((1, d), xt.dtype)], axis=0)
    xb = x_pad[slot_tok].reshape(n_blocks, MOE_BLOCK, d)

    def expert_block(args):
        x_blk, e = args
        return _swiglu(x_blk, w_gate[e], w_up[e], w_down[e])

    yb = lax.map(expert_block, (xb, block_e)).reshape(-1, d)
    y = jnp.zeros_like(xt).at[s_tok].add(yb[dest] * s_gate[:, None].astype(xt.dtype))
    return y.reshape(shp)


def _na_column_layout():
    n_cb = GRID_W // NA_QCB
    q_cols = np.arange(GRID_W).reshape(n_cb, NA_QCB)
    blk_start = np.clip(np.arange(n_cb) * NA_QCB - NA_WIN_COLS // 2, 0, GRID_W - NA_KCB)
    key_cols = blk_start[:, None] + np.arange(NA_KCB)[None, :]
    win_start = np.clip(q_cols - NA_WIN_COLS // 2, 0, GRID_W - NA_WIN_COLS)
    kcol = key_cols[:, None, :]
    ws = win_start[:, :, None]
    mask = (kcol >= ws) & (kcol < ws + NA_WIN_COLS)
    rel_idx = np.clip(kcol - q_cols[:, :, None] + NA_WIN_COLS - 1, 0, 2 * NA_WIN_COLS - 2)
    return key_cols, mask, rel_idx


def _neighbourhood_attention(q, k, v, kc, vc, rpb):
    B, N, H, dh = q.shape
    rows = N // GRID_W
    kr = min(NA_WIN_ROWS, rows)
    n_cb = GRID_W // NA_QCB
    key_cols, col_mask, rel_idx = _na_column_layout()
    qg = (q * dh ** -0.5).reshape(B, rows, GRID_W, H, dh).transpose(1, 0, 3, 2, 4)
    kg = k.reshape(B, rows, GRID_W, H, dh).transpose(0, 3, 1, 2, 4)
    vg = v.reshape(B, rows, GRID_W, H, dh).transpose(0, 3, 1, 2, 4)
    kct = kc.transpose(0, 2, 1, 3)
    vct = vc.transpose(0, 2, 1, 3)
    rpb_cols = rpb[:, :, rel_idx]
    n_win = kr * NA_KCB

    def one_row(args):
        i, qi = args
        rs = jnp.clip(i - kr // 2, 0, rows - kr)
        ks = lax.dynamic_slice_in_dim(kg, rs, kr, axis=2)[:, :, :, key_cols]
        vs = lax.dynamic_slice_in_dim(vg, rs, kr, axis=2)[:, :, :, key_cols]
        qb = qi.reshape(B, H, n_cb, NA_QCB, dh)
        s_win = jnp.einsum('bhcqd,bhrckd->bhcqrk', qb, ks).astype(jnp.float32)
        bias = lax.dynamic_slice_in_dim(rpb_cols, rs - i + NA_WIN_ROWS - 1, kr, axis=1)
        bias = bias.transpose(0, 2, 3, 1, 4).astype(jnp.float32)
        s_win = jnp.where(col_mask[:, :, None, :], s_win + bias, -jnp.inf)
        s_ctx = jnp.einsum('bhcqd,bhld->bhcql', qb, kct).astype(jnp.float32)
        s = jnp.concatenate([s_win.reshape(B, H, n_cb, NA_QCB, n_win), s_ctx], axis=-1)
        p = jax.nn.softmax(s, axis=-1).astype(v.dtype)
        p_win = p[..., :n_win].reshape(B, H, n_cb, NA_QCB, kr, NA_KCB)
        o = (jnp.einsum('bhcqrk,bhrckd->bhcqd', p_win, vs)
             + jnp.einsum('bhcql,bhld->bhcqd', p[..., n_win:], vct))
        return o.reshape(B, H, GRID_W, dh)

    out = lax.map(one_row, (jnp.arange(rows), qg))
    return out.transpose(1, 0, 3, 2, 4).reshape(B, N, H * dh)


def _context_attention(q, k, v):
    B, L, H, dh = q.shape
    s = jnp.einsum('blhd,bmhd->bhlm', q, k).astype(jnp.float32) * (dh ** -0.5)
    p = jax.nn.softmax(s, axis=-1).astype(v.dtype)
    return jnp.einsum('bhlm,bmhd->blhd', p, v).reshape(B, L, H * dh)


def _token_shift(u, mu_prev, mu_next):
    zero = jnp.zeros_like(u[:, :1])
    prev = jnp.concatenate([zero, u[:, :-1]], axis=1)
    nxt = jnp.concatenate([u[:, 1:], zero], axis=1)
    return u + mu_prev * (prev - u) + mu_next * (nxt - u)


def _head_norm(y, g, b):
    mu = jnp.mean(y, axis=-1, keepdims=True)
    var = jnp.mean(jnp.square(y - mu), axis=-1, keepdims=True)
    yn = (y - mu) * lax.rsqrt(var + RW_GN_EPS)
    return yn.reshape(y.shape[0], y.shape[1], -1) * g + b


def _rwkv7_scan(r, w, k, v, z, b):
    G, H, dh = r.shape[1:]

    def step(S, inp):
        r_t, w_t, k_t, v_t, z_t, b_t = inp
        s_z = jnp.einsum('ghvk,ghk->ghv', S, z_t)
        S = S * w_t[:, :, None, :] + s_z[..., None] * b_t[:, :, None, :] + v_t[..., None] * k_t[:, :, None, :]
        return S, jnp.einsum('ghvk,ghk->ghv', S, r_t)

    _, y = lax.scan(step, jnp.zeros((G, H, dh, dh), jnp.float32), (r, w, k, v, z, b))
    return y


def _bi_rwkv7(ul, uc, mu_prev, mu_next, w0, w2, a0, a2, g2, k_k, k_a, r_k, gn_g, gn_b, need_ctx_out):
    B, N, _ = ul.shape
    L = uc.shape[1]
    T = L + N
    u = jnp.concatenate([_token_shift(uc, mu_prev, mu_next), _token_shift(ul, mu_prev, mu_next)],
                        axis=1).astype(jnp.float32)
    heads = lambda t: t.reshape(B, T, RW_HEADS, HEAD_DIM)
    r = u[..., :RW_WIDTH]
    k = u[..., RW_WIDTH:2 * RW_WIDTH]
    v = u[..., 2 * RW_WIDTH:3 * RW_WIDTH]
    kk = heads(k * k_k)
    kk = kk * lax.rsqrt(jnp.sum(kk * kk, axis=-1, keepdims=True) + 1e-12)
    rh, vh = heads(r), heads(v)
    per_dir = []
    for d in range(2):
        lo = 3 * RW_WIDTH + d * RW_LORA
        wd = u[..., lo:lo + RW_DECAY_RANK]
        ad = u[..., lo + RW_DECAY_RANK:lo + RW_DECAY_RANK + RW_A_RANK]
        gd = u[..., lo + RW_DECAY_RANK + RW_A_RANK:lo + RW_LORA]
        log_w = -jax.nn.softplus(-(w0[d] + jnp.tanh(wd) @ w2[d])) - 0.5
        decay = jnp.exp(-jnp.exp(log_w))
        a = jax.nn.sigmoid(a0[d] + ad @ a2[d])
        g = jax.nn.sigmoid(gd) @ g2[d]
        kd = heads(k * (1.0 + (a - 1.0) * k_a))
        per_dir.append((heads(decay), kd, kk * heads(a), g))
    perm = np.concatenate([np.arange(L)[::-1], L + np.arange(N)[::-1]])

    def stack(t_f, t_b):
        return jnp.concatenate([t_f, t_b[:, perm]], axis=0).swapaxes(0, 1)

    (w_f, k_f, b_f, _), (w_b, k_b, b_b, _) = per_dir
    y = _rwkv7_scan(stack(rh, rh), stack(w_f, w_b), stack(k_f, k_b), stack(vh, vh),
                    stack(-kk, -kk), stack(b_f, b_b)).swapaxes(0, 1)
    t0 = 0 if need_ctx_out else L
    y_dirs = (y[:B, t0:], y[B:][:, perm][:, t0:])
    outs = []
    for (_, kd, _, g), yd in zip(per_dir, y_dirs):
        bonus = (jnp.sum(rh * kd * r_k, axis=-1, keepdims=True) * vh)[:, t0:].reshape(B, T - t0, RW_WIDTH)
        outs.append((_head_norm(yd, gn_g, gn_b) + bonus) * g[:, t0:])
    out = (outs[0] + outs[1]).astype(ul.dtype)
    out_l = out[:, L - t0:]
    out_c = out[:, :L] if need_ctx_out else None
    return out_l, out_c


def _conformer_conv(u, dw_w, dw_b, ln_g, ln_b):
    h = u[..., :CV_WIDTH] * jax.nn.sigmoid(u[..., CV_WIDTH:])
    h = lax.conv_general_dilated(h, dw_w[:, None, :], (1,), [(CV_CONV_LEN // 2, CV_CONV_LEN // 2)],
                                 dimension_numbers=('NWC', 'WIO', 'NWC'),
                                 feature_group_count=CV_WIDTH) + dw_b
    return jax.nn.silu(_layernorm(h, ln_g, ln_b))


def _mixers(hl, hc, w_in, w_out, na_rpb, rw_mu_prev, rw_mu_next, rw_w0, rw_w2, rw_a0, rw_a2, rw_g2,
            rw_k_k, rw_k_a, rw_r_k, rw_gn_g, rw_gn_b, cv_dw_w, cv_dw_b, cv_ln_g, cv_ln_b, need_ctx_out):
    o_rw = 3 * NA_WIDTH
    o_cv = o_rw + RW_IN
    heads = lambda t: t.reshape(t.shape[0], t.shape[1], NA_HEADS, HEAD_DIM)
    pl = hl @ w_in
    c_off = 0 if need_ctx_out else NA_WIDTH
    pc = hc @ (w_in if need_ctx_out else w_in[:, NA_WIDTH:o_cv])
    kc = heads(pc[..., NA_WIDTH - c_off:2 * NA_WIDTH - c_off])
    vc = heads(pc[..., 2 * NA_WIDTH - c_off:o_rw - c_off])
    na_l = _neighbourhood_attention(heads(pl[..., :NA_WIDTH]), heads(pl[..., NA_WIDTH:2 * NA_WIDTH]),
                                    heads(pl[..., 2 * NA_WIDTH:o_rw]), kc, vc, na_rpb)
    rw_l, rw_c = _bi_rwkv7(pl[..., o_rw:o_cv], pc[..., o_rw - c_off:o_cv - c_off], rw_mu_prev, rw_mu_next,
                           rw_w0, rw_w2, rw_a0, rw_a2, rw_g2, rw_k_k, rw_k_a, rw_r_k, rw_gn_g, rw_gn_b,
                           need_ctx_out)
    cv_l = _conformer_conv(pl[..., o_cv:], cv_dw_w, cv_dw_b, cv_ln_g, cv_ln_b)
    out_l = jnp.concatenate([na_l, rw_l, cv_l], axis=-1) @ w_out
    if not need_ctx_out:
        return out_l, None
    na_c = _context_attention(heads(pc[..., :NA_WIDTH]), kc, vc)
    cv_c = _conformer_conv(pc[..., o_cv:], cv_dw_w, cv_dw_b, cv_ln_g, cv_ln_b)
    out_c = jnp.concatenate([na_c, rw_c, cv_c], axis=-1) @ w_out
    return out_l, out_c


def setup_inputs(seed: int = 0) -> dict:
    key = jax.random.key(seed)
    ks = iter(jax.random.split(key, 40))
    f32 = jnp.float32

    def nrm(shape, std):
        return jax.random.normal(next(ks), shape, f32) * std

    def uni(shape, lo, hi):
        return jax.random.uniform(next(ks), shape, f32, lo, hi)

    D = D_MODEL
    return {
        'x': nrm((BATCH, SEQ, D), 1.0),
        'c': nrm((BATCH, D), 1.0),
        'ctx': nrm((BATCH, CTX_LEN, D), 1.0),
        'c_ctx': nrm((D,), 1.0),
        'norm1_g': 1.0 + nrm((DEPTH, D), 0.02),
        'norm2_g': 1.0 + nrm((DEPTH, D), 0.02),
        'mod_w': nrm((DEPTH, D, 6 * D), 0.5 * D ** -0.5),
        'mod_b': nrm((DEPTH, 6 * D), 0.02),
        'w_in': nrm((DEPTH, D, IN_WIDTH), D ** -0.5),
        'w_out': nrm((DEPTH, MIX_WIDTH, D), MIX_WIDTH ** -0.5),
        'na_rpb': nrm((DEPTH, NA_HEADS, 2 * NA_WIN_ROWS - 1, 2 * NA_WIN_COLS - 1), 0.1),
        'rw_mu_prev': uni((DEPTH, RW_IN), 0.0, 0.5),
        'rw_mu_next': uni((DEPTH, RW_IN), 0.0, 0.5),
        'rw_w0': uni((DEPTH, 2, RW_WIDTH), -6.0, 1.0),
        'rw_w2': nrm((DEPTH, 2, RW_DECAY_RANK, RW_WIDTH), 0.5 * RW_DECAY_RANK ** -0.5),
        'rw_a0': nrm((DEPTH, 2, RW_WIDTH), 0.3),
        'rw_a2': nrm((DEPTH, 2, RW_A_RANK, RW_WIDTH), 0.5 * RW_A_RANK ** -0.5),
        'rw_g2': nrm((DEPTH, 2, RW_GATE_RANK, RW_WIDTH), RW_GATE_RANK ** -0.5),
        'rw_k_k': 0.85 + nrm((DEPTH, RW_WIDTH), 0.02),
        'rw_k_a': 1.0 + nrm((DEPTH, RW_WIDTH), 0.02),
        'rw_r_k': nrm((DEPTH, RW_HEADS, HEAD_DIM), 0.1),
        'rw_gn_g': 1.0 + nrm((DEPTH, RW_WIDTH), 0.02),
        'rw_gn_b': nrm((DEPTH, RW_WIDTH), 0.02),
        'cv_dw_w': nrm((DEPTH, CV_CONV_LEN, CV_WIDTH), CV_CONV_LEN ** -0.5),
        'cv_dw_b': nrm((DEPTH, CV_WIDTH), 0.02),
        'cv_ln_g': 1.0 + nrm((DEPTH, CV_WIDTH), 0.02),
        'cv_ln_b': nrm((DEPTH, CV_WIDTH), 0.02),
        'ffn_w_gate': nrm((N_DENSE_LAYERS, D, D_FF), D ** -0.5),
        'ffn_w_up': nrm((N_DENSE_LAYERS, D, D_FF), D ** -0.5),
        'ffn_w_down': nrm((N_DENSE_LAYERS, D_FF, D), D_FF ** -0.5),
        'moe_router': nrm((N_MOE_LAYERS, D, N_EXPERTS), D ** -0.5),
        'moe_w_gate': nrm((N_MOE_LAYERS, N_EXPERTS, D, D_FF_EXPERT), D ** -0.5),
        'moe_w_up': nrm((N_MOE_LAYERS, N_EXPERTS, D, D_FF_EXPERT), D ** -0.5),
        'moe_w_down': nrm((N_MOE_LAYERS, N_EXPERTS, D_FF_EXPERT, D), D_FF_EXPERT ** -0.5),
        'final_g': 1.0 + nrm((D,), 0.02),
    }


def reference(x, c, ctx, c_ctx, norm1_g, norm2_g, mod_w, mod_b, w_in, w_out, na_rpb, rw_mu_prev, rw_mu_next,
              rw_w0, rw_w2, rw_a0, rw_a2, rw_g2, rw_k_k, rw_k_a, rw_r_k, rw_gn_g, rw_gn_b, cv_dw_w, cv_dw_b,
              cv_ln_g, cv_ln_b, ffn_w_gate, ffn_w_up, ffn_w_down, moe_router, moe_w_gate, moe_w_up, moe_w_down,
              final_g):
    c_sil = jax.nn.silu(c)
    cc_sil = jax.nn.silu(c_ctx)
    xl, xc = x, ctx
    for layer in range(DEPTH):
        need_ctx = layer < DEPTH - 1
        sh1, sc1, g1, sh2, sc2, g2 = jnp.split((c_sil @ mod_w[layer] + mod_b[layer])[:, None, :], 6, axis=-1)
        csh1, csc1, cg1, csh2, csc2, cg2 = jnp.split(cc_sil @ mod_w[layer] + mod_b[layer], 6, axis=-1)
        hl = _modulate(_rmsnorm(xl, norm1_g[layer]), sh1, sc1)
        hc = _modulate(_rmsnorm(xc, norm1_g[layer]), csh1, csc1)
        mix_l, mix_c = _mixers(hl, hc, w_in[layer], w_out[layer], na_rpb[layer], rw_mu_prev[layer],
                               rw_mu_next[layer], rw_w0[layer], rw_w2[layer], rw_a0[layer], rw_a2[layer],
                               rw_g2[layer], rw_k_k[layer], rw_k_a[layer], rw_r_k[layer], rw_gn_g[layer],
                               rw_gn_b[layer], cv_dw_w[layer], cv_dw_b[layer], cv_ln_g[layer], cv_ln_b[layer],
                               need_ctx)
        xl = xl + g1 * mix_l
        hl = _modulate(_rmsnorm(xl, norm2_g[layer]), sh2, sc2)
        j = layer // 2
        if layer % 2 == 0:
            xl = xl + g2 * _swiglu(hl, ffn_w_gate[j], ffn_w_up[j], ffn_w_down[j])
        else:
            xl = xl + g2 * _moe_swiglu(hl, moe_router[j], moe_w_gate[j], moe_w_up[j], moe_w_down[j])
        if need_ctx:
            xc = xc + cg1 * mix_c
            hc = _modulate(_rmsnorm(xc, norm2_g[layer]), csh2, csc2)
            if layer % 2 == 0:
                xc = xc + cg2 * _swiglu(hc, ffn_w_gate[j], ffn_w_up[j], ffn_w_down[j])
            else:
                xc = xc + cg2 * _moe_swiglu(hc, moe_router[j], moe_w_gate[j], moe_w_up[j], moe_w_down[j])
    return _rmsnorm(xl, final_g)
```

```python
import contextlib
import numpy as np
import concourse.bass as bass
import concourse.mybir as mybir
from concourse.bass_utils import run_bass_kernel_spmd

F32 = mybir.dt.float32
BF16 = mybir.dt.bfloat16
I32 = mybir.dt.int32
U32 = mybir.dt.uint32
AF = mybir.ActivationFunctionType
ALU = mybir.AluOpType
AX = mybir.AxisListType

ENGS = ['pe', 'dve', 'act', 'pool', 'sp']
NDS = 6


class Buf:
    def __init__(self, t, name, excl=False):
        self.t = t
        self.name = name
        self.excl = excl
        self.w = []
        self.r = []

    def __getitem__(self, idx):
        return self.t[idx]


class Prog:
    def __init__(self, nc, stack):
        self.nc = nc
        self.stack = stack
        self.q = {e: [] for e in ENGS}
        self.sem = {e: stack.enter_context(nc.semaphore("s_" + e)) for e in ENGS}
        self.dsem = {qe: [stack.enter_context(nc.semaphore("d_%s_%d" % (qe, i))) for i in range(NDS)]
                     for qe in ['sp', 'act', 'pool']}
        self.dcnt = {qe: [0] * NDS for qe in ['sp', 'act', 'pool']}
        self.drr = {qe: 0 for qe in ['sp', 'act', 'pool']}
        self.dk = {}
        self.nbuf = 0

    def sb(self, shape, dt=F32, name=None, stack=None):
        self.nbuf += 1
        name = name or "sb%d" % self.nbuf
        t = (stack or self.stack).enter_context(self.nc.sbuf_tensor(name, list(shape), dt))
        return Buf(t, name)

    def ps(self, shape, dt=F32, name=None):
        self.nbuf += 1
        name = name or "ps%d" % self.nbuf
        t = self.stack.enter_context(self.nc.psum_tensor(name, list(shape), dt))
        return Buf(t, name, excl=True)

    def dkey(self, *key):
        if key not in self.dk:
            self.dk[key] = Buf(None, str(key))
        return self.dk[key]

    def _deps(self, reads, writes, eng=None):
        deps = []
        for b in reads:
            deps.extend(b.w)
            if b.excl:
                deps.extend(d for d in b.r if not (d[0] == 'e' and d[1] == eng))
        for b in writes:
            deps.extend(b.w)
            deps.extend(b.r)
        return deps

    def _commit(self, ev, reads, writes):
        for b in reads:
            b.r.append(ev)
        for b in writes:
            b.w = [ev]
            b.r = []

    def op(self, eng, fn, reads=(), writes=()):
        deps = self._deps(reads, writes, eng)
        idx = len(self.q[eng])
        ev = ('e', eng, idx)
        self.q[eng].append([deps, fn, 'op', None])
        self._commit(ev, reads, writes)
        return ev

    def dma(self, qe, out, in_, reads=(), writes=(), **kw):
        deps = self._deps(reads, writes)
        k = self.drr[qe]
        self.drr[qe] = (k + 1) % NDS
        prev = self.dcnt[qe][k]
        if prev > 0:
            deps.append(('d', qe, k, prev))
        self.dcnt[qe][k] = prev + 16
        ev = ('d', qe, k, prev + 16)
        self.q[qe].append([deps, (out, in_, kw), 'dma', (qe, k)])
        self._commit(ev, reads, writes)
        return ev

    def finalize(self):
        nc = self.nc
        waited = {e: set() for e in ENGS}
        for e in ENGS:
            for deps, fn, kind, x in self.q[e]:
                for d in deps:
                    if d[0] == 'e':
                        if d[1] == 'pe' and e == 'pe':
                            continue
                        waited[d[1]].add(d[2])
        rank = {}
        for e in ENGS:
            s = sorted(waited[e])
            rank[e] = {i: k + 1 for k, i in enumerate(s)}
        self.ninc = {e: len(waited[e]) for e in ENGS}
        eobj = {'pe': nc.tensor, 'dve': nc.vector, 'act': nc.scalar, 'pool': nc.gpsimd, 'sp': nc.sync}

        def replay(e, engine):
            seen = {}
            for idx, (deps, fn, kind, x) in enumerate(self.q[e]):
                need = {}
                for d in deps:
                    if d[0] == 'e':
                        if d[1] == 'pe' and e == 'pe':
                            continue
                        key = ('e', d[1])
                        val = rank[d[1]][d[2]]
                    else:
                        key = ('d', d[1], d[2])
                        val = d[3]
                    if seen.get(key, 0) >= val:
                        continue
                    if need.get(key, 0) < val:
                        need[key] = val
                for key, val in need.items():
                    seen[key] = val
                    sem = self.sem[key[1]] if key[0] == 'e' else self.dsem[key[1]][key[2]]
                    engine.wait_ge(sem, val)
                if kind == 'op':
                    ins = fn(engine)
                    if idx in rank[e]:
                        ins.then_inc(self.sem[e], 1)
                else:
                    out, in_, kw = fn
                    qe, k = x
                    engine.dma_start(out=out, in_=in_, **kw).then_inc(self.dsem[qe][k], 16)

        with nc.Block() as block:
            @block.tensor
            def _(eng):
                replay('pe', eng)

            @block.vector
            def _(eng):
                replay('dve', eng)

            @block.scalar
            def _(eng):
                replay('act', eng)

            @block.gpsimd
            def _(eng):
                replay('pool', eng)

            @block.sync
            def _(eng):
                replay('sp', eng)

    def wait_all(self, eng, bufs):
        deps = []
        for b in bufs:
            deps.extend(b.w)
        self.q[eng].append([deps, lambda e: e.nop(), 'op', None])


D = 1024
LC = 256
NL = 4096
T = LC + NL
NT = T // 128
HTW = T + 4
INW = 3328
RW_IN = 1280
EPS_RMS = 1e-6


def htcol(t):
    return 1 + t if t < LC else 3 + t


class K:
    def __init__(self, nc, stack, ins, dbg=()):
        self.nc = nc
        self.dbg = set(dbg)
        self.P = Prog(nc, stack)
        self.ins = ins
        self.banks = [self.P.ps([128, 512], F32, name="bank%d" % i) for i in range(8)]
        self.dr = {}
        P = self.P
        self.eps_rms = P.sb([128, 1], F32, name="eps_rms")
        P.op('pool', lambda e: e.memset(self.eps_rms[:], EPS_RMS), writes=[self.eps_rms])
        self.ident = P.sb([128, 128], F32, name="ident")
        P.op('pool', lambda e: e.memset(self.ident[:], 1.0), writes=[self.ident])
        P.op('pool', lambda e: e.affine_select(out=self.ident[:], in_=self.ident[:], pattern=[[-1, 128]],
                                               compare_op=ALU.is_equal, fill=0.0, base=0, channel_multiplier=1),
             reads=[self.ident], writes=[self.ident])

    def dram(self, name, shape, dt):
        if name not in self.dr:
            kind = "ExternalOutput" if name in self.dbg else "Internal"
            self.dr[name] = self.nc.dram_tensor(name, list(shape), dt, kind=kind).ap()
        return self.dr[name]

    def barrier(self):
        P = self.P
        evs = []
        for e in ['pe', 'dve', 'act', 'pool']:
            for idx in range(len(P.q[e]) - 1, -1, -1):
                if P.q[e][idx][2] == 'op' and P.q[e][idx][3] != 'nop':
                    evs.append(('e', e, idx))
                    break
        for qe in ['sp', 'act', 'pool']:
            for k in range(NDS):
                if P.dcnt[qe][k] > 0:
                    evs.append(('d', qe, k, P.dcnt[qe][k]))
        for e in ENGS:
            P.q[e].append([list(evs), (lambda en: en.nop()), 'op', 'nop'])


def phase_mod(k, l, ph):
    P, nc, ins = k.P, k.nc, k.ins
    modrep = k.dram("modrep", [2, 6, 128, 1024], F32)
    cT = P.sb([128, 2, 8], F32, stack=ph)
    P.dma('sp', cT[:, 0, :], ins['cT'], writes=[cT])
    P.dma('sp', cT[:, 1, :], ins['cctxT'], writes=[cT])
    cs = P.sb([128, 2, 8], F32, stack=ph)
    P.op('act', lambda e: e.activation(out=cs[:], in_=cT[:], func=AF.Silu), reads=[cT], writes=[cs])
    csr = P.sb([128, 8, 2, 64], F32, stack=ph)
    P.op('dve', lambda e: e.tensor_copy(out=csr[:], in_=cs[:].rearrange("p w k -> p k w").unsqueeze(3).broadcast_to(
        [128, 8, 2, 64])), reads=[cs], writes=[csr])
    wm = [P.sb([128, 8, 512], F32, stack=ph) for _ in range(2)]
    brep = [P.sb([128, 512], F32, stack=ph) for _ in range(2)]
    grep = [P.sb([128, 512], F32, stack=ph) for _ in range(2)]
    res = [P.sb([128, 512], F32, stack=ph) for _ in range(4)]
    for cc in range(12):
        which, half = cc // 2, cc % 2
        w_, b_, g_ = wm[cc % 2], brep[cc % 2], grep[cc % 2]
        P.dma('sp', w_[:], ins['mod_w'][l][:, cc * 512:(cc + 1) * 512].rearrange("(kt p) n -> p kt n", p=128),
              writes=[w_])
        P.dma('sp', b_[:], ins['mod_b'][l][cc * 512:(cc + 1) * 512].partition_broadcast(128), writes=[b_])
        if which in (1, 4):
            gsrc = ins['norm1_g'] if which == 1 else ins['norm2_g']
            P.dma('sp', g_[:], gsrc[l][half * 512:(half + 1) * 512].partition_broadcast(128), writes=[g_])
        bk = k.banks[cc % 4]
        for kt in range(8):
            P.op('pe', lambda e, kt=kt, w_=w_, bk=bk: e.matmul(
                bk[:, :], lhsT=csr[:, kt, :, :].rearrange("p w j -> p (w j)"), rhs=w_[:, kt, :], start=(kt == 0),
                stop=(kt == 7)), reads=[csr, w_], writes=[bk])
        r_ = res[cc % 4]
        P.op('dve', lambda e, r_=r_, bk=bk, b_=b_: e.tensor_tensor(out=r_[:], in0=bk[:, :], in1=b_[:], op=ALU.add),
             reads=[bk, b_], writes=[r_])
        if which in (1, 4):
            P.op('dve', lambda e, r_=r_, g_=g_: e.scalar_tensor_tensor(
                out=r_[:], in0=r_[:], scalar=1.0, in1=g_[:], op0=ALU.add, op1=ALU.mult),
                reads=[r_, g_], writes=[r_])
        for who in range(2):
            for hp in range(2):
                P.dma('act' if hp else 'sp', modrep[who, which, hp * 64:(hp + 1) * 64, half * 512:(half + 1) * 512],
                      r_[who * 64:(who + 1) * 64, :], reads=[r_], writes=[P.dkey('mod', who, which, half, hp)])


def load_modrep(k, which, ph, eng='sp'):
    P = k.P
    modrep = k.dram("modrep", [2, 6, 128, 1024], F32)
    out = []
    for who in range(2):
        t = P.sb([128, 1024], F32, stack=ph)
        P.dma(eng, t[:], modrep[who, which], reads=[P.dkey('mod', who, which, h_, p_) for h_ in range(2) for p_ in range(2)],
              writes=[t])
        out.append(t)
    return out


def rms_rstd(k, xt, junk, ss, rstd):
    P = k.P
    P.op('act', lambda e: e.activation(out=junk[:], in_=xt[:], func=AF.Square, accum_out=ss[:]),
         reads=[xt], writes=[junk, ss])
    P.op('act', lambda e: e.activation(out=rstd[:], in_=ss[:], func=AF.Ln, scale=1.0 / D, bias=k.eps_rms[:]),
         reads=[ss, k.eps_rms], writes=[rstd])
    P.op('act', lambda e: e.activation(out=rstd[:], in_=rstd[:], func=AF.Exp, scale=-0.5),
         reads=[rstd], writes=[rstd])


def phase_a(k, l, ph, xsrc, xkeyfn, HT):
    P = k.P
    gm = load_modrep(k, 1, ph)
    sh = load_modrep(k, 0, ph)
    for c in (0, LC + 1, LC + 2, HTW - 1):
        P.op('pool', lambda e, c=c: e.memset(HT[:, :, c:c + 1], 0.0), writes=[HT])
    xt = [P.sb([128, 1024], F32, stack=ph) for _ in range(2)]
    xh = [P.sb([128, 1024], F32, stack=ph) for _ in range(2)]
    junk = P.sb([128, 1024], F32, stack=ph)
    ss = [P.sb([128, 1], F32, stack=ph) for _ in range(2)]
    rstd = [P.sb([128, 1], F32, stack=ph) for _ in range(2)]
    def stage1(i):
        who = 1 if i < 2 else 0
        x_, h_, s_, r_ = xt[i % 2], xh[i % 2], ss[i % 2], rstd[i % 2]
        P.dma('sp', x_[:], xsrc[i * 128:(i + 1) * 128, :], reads=xkeyfn(i), writes=[x_])
        rms_rstd(k, x_, junk, s_, r_)
        P.op('dve', lambda e, x_=x_, h_=h_, r_=r_, who=who: e.scalar_tensor_tensor(
            out=h_[:], in0=x_[:], scalar=r_[:, 0:1], in1=gm[who][:], op0=ALU.mult, op1=ALU.mult),
            reads=[x_, r_, gm[who]], writes=[h_])
        P.op('pool', lambda e, h_=h_, who=who: e.tensor_tensor(out=h_[:], in0=h_[:], in1=sh[who][:], op=ALU.add),
             reads=[h_, sh[who]], writes=[h_])

    def stage2(i):
        h_ = xh[i % 2]
        c0 = htcol(i * 128)
        for hb in range(2):
            bk = k.banks[(i % 2) * 2 + hb]
            for j in range(4):
                f = hb * 4 + j
                P.op('pe', lambda e, bk=bk, j=j, f=f, h_=h_: e.transpose(
                    out=bk[:, j * 128:(j + 1) * 128], in_=h_[:, f * 128:(f + 1) * 128], identity=k.ident[:]),
                    reads=[h_, k.ident], writes=[bk])
            if hb == 0:
                P.op('act', lambda e, bk=bk, hb=hb, c0=c0: e.activation(
                    out=HT[:, hb * 4:(hb + 1) * 4, c0:c0 + 128], in_=bk[:, :].rearrange("p (j t) -> p j t", j=4),
                    func=AF.Copy), reads=[bk], writes=[HT])
            else:
                P.op('dve', lambda e, bk=bk, hb=hb, c0=c0: e.tensor_copy(
                    out=HT[:, hb * 4:(hb + 1) * 4, c0:c0 + 128], in_=bk[:, :].rearrange("p (j t) -> p j t", j=4)),
                    reads=[bk], writes=[HT])

    stage1(0)
    for i in range(NT):
        if i + 1 < NT:
            stage1(i + 1)
        stage2(i)


def load_cast(k, g, dst, src2d, ncols):
    P = k.P
    stg = [P.sb([128, ncols], F32, stack=g) for _ in range(3)]
    for kt in range(8):
        b = stg[kt % 3]
        P.dma('sp', b[:], src2d[kt * 128:(kt + 1) * 128, :], writes=[b])
        o = dst[:, kt, :]
        if kt % 2 == 0:
            P.op('dve', lambda e, o=o, b=b: e.tensor_copy(out=o, in_=b[:]), reads=[b], writes=[dst])
        else:
            P.op('act', lambda e, o=o, b=b: e.activation(out=o, in_=b[:], func=AF.Copy), reads=[b], writes=[dst])


def tok_blocks():
    return [(0, LC)] + [(LC + b * 512, 512) for b in range(NL // 512)]


def phase_b(k, l, ph, HT):
    P, ins = k.P, k.ins
    qkT = k.dram("qkT", [1024, T], BF16)
    vtm = k.dram("vtm", [T, 512], BF16)
    utm = k.dram("utm", [T, RW_IN], F32)
    gluT = k.dram("gluT", [256, HTW], BF16)
    w_in = ins['w_in'][l]
    wv3 = lambda c0, c1: w_in[:, c0:c1].rearrange("(kt p) n -> p kt n", p=128)
    with contextlib.ExitStack() as g:
        wqk = P.sb([128, 8, 1024], BF16, stack=g)
        load_cast(k, g, wqk, w_in[:, 0:1024], 1024)
        stg = [P.sb([128, 512], BF16, stack=g) for _ in range(3)]
        n_ = 0
        for (t0, n) in tok_blocks():
            c0 = htcol(t0)
            for ft in range(8):
                bk = k.banks[n_ % 4]
                s_ = stg[n_ % 3]
                n_ += 1
                for kt in range(8):
                    P.op('pe', lambda e, bk=bk, kt=kt, ft=ft, c0=c0, n=n: e.matmul(
                        bk[:, 0:n], lhsT=wqk[:, kt, ft * 128:(ft + 1) * 128], rhs=HT[:, kt, c0:c0 + n],
                        start=(kt == 0), stop=(kt == 7)), reads=[wqk, HT], writes=[bk])
                eng = 'act' if n_ % 2 == 0 else 'dve'
                if eng == 'act':
                    P.op('act', lambda e, bk=bk, s_=s_, n=n: e.activation(out=s_[:, 0:n], in_=bk[:, 0:n], func=AF.Copy),
                         reads=[bk], writes=[s_])
                else:
                    P.op('dve', lambda e, bk=bk, s_=s_, n=n: e.tensor_copy(out=s_[:, 0:n], in_=bk[:, 0:n]),
                         reads=[bk], writes=[s_])
                P.dma('sp', qkT[ft * 128:(ft + 1) * 128, t0:t0 + n], s_[:, 0:n], reads=[s_],
                      writes=[P.dkey('qkT', ft, t0)])
        k.barrier()
    with contextlib.ExitStack() as g:
        wv = P.sb([128, 8, 512], BF16, stack=g)
        load_cast(k, g, wv, w_in[:, 1024:1536], 512)
        stg = [P.sb([128, 512], BF16, stack=g) for _ in range(3)]
        for i in range(NT):
            c0 = htcol(i * 128)
            bk = k.banks[i % 4]
            s_ = stg[i % 3]
            for kt in range(8):
                P.op('pe', lambda e, bk=bk, kt=kt, c0=c0: e.matmul(
                    bk[:, :], lhsT=HT[:, kt, c0:c0 + 128], rhs=wv[:, kt, :], start=(kt == 0), stop=(kt == 7)),
                    reads=[wv, HT], writes=[bk])
            if i % 2 == 0:
                P.op('act', lambda e, bk=bk, s_=s_: e.activation(out=s_[:], in_=bk[:, :], func=AF.Copy),
                     reads=[bk], writes=[s_])
            else:
                P.op('dve', lambda e, bk=bk, s_=s_: e.tensor_copy(out=s_[:], in_=bk[:, :]), reads=[bk], writes=[s_])
            P.dma('sp', vtm[i * 128:(i + 1) * 128, :], s_[:], reads=[s_], writes=[P.dkey('vtm', i)])
        k.barrier()
    with contextlib.ExitStack() as g:
        wrb = P.sb([128, 8, RW_IN], BF16, stack=g)
        load_cast(k, g, wrb, w_in[:, 1536:1536 + RW_IN], RW_IN)
        muT = P.sb([128, 10, 2], F32, stack=g)
        P.dma('sp', muT[:], ins['rw_muT'][l], writes=[muT])
        c0T = P.sb([128, 10, 1], F32, stack=g)
        P.op('dve', lambda e: e.tensor_tensor(out=c0T[:], in0=muT[:, :, 0:1], in1=muT[:, :, 1:2], op=ALU.add),
             reads=[muT], writes=[c0T])
        P.op('dve', lambda e: e.tensor_scalar(out=c0T[:], in0=c0T[:], scalar1=-1.0, scalar2=1.0, op0=ALU.mult,
                                              op1=ALU.add), reads=[c0T], writes=[c0T])
        uT = [P.sb([128, 10, 384], F32, stack=g) for _ in range(2)]
        stg = [P.sb([128, RW_IN], F32, stack=g) for _ in range(2)]
        rblocks = [(0, LC)] + [(LC + 384 * j, 384) for j in range(10)] + [(LC + 3840, 256)]
        tn_ = 0
        for bi, (t0, n) in enumerate(rblocks):
            u_ = uT[bi % 2]
            cc = htcol(t0)
            for ft in range(10):
                bk = k.banks[ft % 4]
                for kt in range(8):
                    P.op('pe', lambda e, bk=bk, kt=kt, ft=ft, cc=cc, n=n: e.matmul(
                        bk[:, 0:n + 2], lhsT=wrb[:, kt, ft * 128:(ft + 1) * 128], rhs=HT[:, kt, cc - 1:cc + n + 1],
                        start=(kt == 0), stop=(kt == 7)), reads=[wrb, HT], writes=[bk])
                P.op('dve', lambda e, bk=bk, u_=u_, ft=ft, n=n: e.tensor_scalar(
                    out=u_[:, ft, 0:n], in0=bk[:, 1:n + 1], scalar1=c0T[:, ft, 0:1], scalar2=None, op0=ALU.mult),
                    reads=[bk, c0T], writes=[u_])
                P.op('dve', lambda e, bk=bk, u_=u_, ft=ft, n=n: e.scalar_tensor_tensor(
                    out=u_[:, ft, 0:n], in0=bk[:, 0:n], scalar=muT[:, ft, 0:1], in1=u_[:, ft, 0:n], op0=ALU.mult,
                    op1=ALU.add), reads=[bk, muT, u_], writes=[u_])
                P.op('dve', lambda e, bk=bk, u_=u_, ft=ft, n=n: e.scalar_tensor_tensor(
                    out=u_[:, ft, 0:n], in0=bk[:, 2:n + 2], scalar=muT[:, ft, 1:2], in1=u_[:, ft, 0:n], op0=ALU.mult,
                    op1=ALU.add), reads=[bk, muT, u_], writes=[u_])
            for j in range(n // 128):
                i = t0 // 128 + j
                s_ = stg[i % 2]
                for (f0, f1) in ((0, 4), (4, 8), (8, 10)):
                    bk = k.banks[4 + tn_ % 4]
                    tn_ += 1
                    for ft in range(f0, f1):
                        P.op('pe', lambda e, bk=bk, ft=ft, f0=f0, u_=u_, j=j: e.transpose(
                            out=bk[:, (ft - f0) * 128:(ft - f0 + 1) * 128], in_=u_[:, ft, j * 128:(j + 1) * 128],
                            identity=k.ident[:]), reads=[u_, k.ident], writes=[bk])
                    w = (f1 - f0) * 128
                    if tn_ % 2 == 0:
                        P.op('act', lambda e, bk=bk, s_=s_, f0=f0, w=w: e.activation(
                            out=s_[:, f0 * 128:f0 * 128 + w], in_=bk[:, 0:w], func=AF.Copy), reads=[bk], writes=[s_])
                    else:
                        P.op('pool' if False else 'dve', lambda e, bk=bk, s_=s_, f0=f0, w=w: e.tensor_copy(
                            out=s_[:, f0 * 128:f0 * 128 + w], in_=bk[:, 0:w]), reads=[bk], writes=[s_])
                P.dma('sp', utm[i * 128:(i + 1) * 128, :], s_[:], reads=[s_], writes=[P.dkey('utm', i)])
        k.barrier()
    with contextlib.ExitStack() as g:
        wc = P.sb([128, 8, 512], BF16, stack=g)
        load_cast(k, g, wc, w_in[:, 1536 + RW_IN:INW], 512)
        sig = [P.sb([128, 512], F32, stack=g) for _ in range(2)]
        stg = [P.sb([128, 512], BF16, stack=g) for _ in range(3)]
        n_ = 0
        for (t0, n) in tok_blocks():
            c0 = htcol(t0)
            for j in range(2):
                ba, bb = k.banks[(n_ % 2) * 2], k.banks[(n_ % 2) * 2 + 1]
                sg, s_ = sig[n_ % 2], stg[n_ % 3]
                n_ += 1
                for (bk, fo) in ((ba, j * 128), (bb, 256 + j * 128)):
                    for kt in range(8):
                        P.op('pe', lambda e, bk=bk, kt=kt, fo=fo, c0=c0, n=n: e.matmul(
                            bk[:, 0:n], lhsT=wc[:, kt, fo:fo + 128], rhs=HT[:, kt, c0:c0 + n],
                            start=(kt == 0), stop=(kt == 7)), reads=[wc, HT], writes=[bk])
                P.op('act', lambda e, bb=bb, sg=sg, n=n: e.activation(out=sg[:, 0:n], in_=bb[:, 0:n], func=AF.Sigmoid),
                     reads=[bb], writes=[sg])
                P.op('dve', lambda e, ba=ba, sg=sg, s_=s_, n=n: e.tensor_tensor(
                    out=s_[:, 0:n], in0=ba[:, 0:n], in1=sg[:, 0:n], op=ALU.mult), reads=[ba, sg], writes=[s_])
                P.dma('sp', gluT[j * 128:(j + 1) * 128, c0:c0 + n], s_[:, 0:n], reads=[s_],
                      writes=[P.dkey('gluT', j, t0)])
        k.barrier()


LN_EPS = 1e-5
GSW = 15 + LC + 30 + NL + 15
SEG = [(0, LC, 15), (LC, NL, 15 + LC + 30)]
NEG = -30000.0


def phase_c(k, l, ph):
    P, ins = k.P, k.ins
    gluT = k.dram("gluT", [256, HTW], BF16)
    mixT = k.dram("mixT", [1024, T], BF16)
    GS = P.sb([128, 2, GSW], BF16, stack=ph)
    P.op('pool', lambda e: e.memset(GS[:], 0.0), writes=[GS])
    for ct in range(2):
        for (t0, n, off) in SEG:
            P.dma('sp', GS[:, ct, off:off + n], gluT[ct * 128:(ct + 1) * 128, htcol(t0):htcol(t0) + n],
                  reads=[b for kk, b in P.dk.items() if kk[0] == 'gluT' and kk[1] == ct], writes=[GS])
    dwT = P.sb([128, 2, 31], F32, stack=ph)
    P.dma('sp', dwT[:], ins['cv_dw_wT'][l].rearrange("(ct p) j -> p ct j", p=128), writes=[dwT])
    cvp = P.sb([128, 2, 3], F32, stack=ph)
    P.dma('sp', cvp[:], ins['cvp'][l], writes=[cvp])
    identb = P.sb([128, 128], BF16, stack=ph)
    P.op('dve', lambda e: e.tensor_copy(out=identb[:], in_=k.ident[:]), reads=[k.ident], writes=[identb])
    DW = P.sb([128, 2, 31, 128], BF16, stack=ph)
    n_ = 0
    for ct in range(2):
        for j in range(31):
            eng = 'dve' if n_ % 2 == 0 else 'pool'
            n_ += 1
            P.op(eng, lambda e, ct=ct, j=j: e.tensor_scalar(out=DW[:, ct, j, :], in0=identb[:],
                                                            scalar1=dwT[:, ct, j:j + 1], scalar2=None, op0=ALU.mult),
                 reads=[identb, dwT], writes=[DW])
    onesM = P.sb([128, 128], F32, stack=ph)
    P.op('pool', lambda e: e.memset(onesM[:], 1.0 / 256), writes=[onesM])
    epsln = P.sb([128, 1], F32, stack=ph)
    P.op('pool', lambda e: e.memset(epsln[:], LN_EPS), writes=[epsln])
    hcs = [P.sb([128, 2, 512], F32, stack=ph) for _ in range(2)]
    sqs = [P.sb([128, 2, 512], F32, stack=ph) for _ in range(2)]
    m2 = P.sb([128, 512], F32, stack=ph)
    var = P.sb([128, 512], F32, stack=ph)
    dd = [P.sb([128, 512], F32, stack=ph) for _ in range(2)]
    stg = [P.sb([128, 512], BF16, stack=ph) for _ in range(3)]
    bi = 0
    si = 0
    for (st0, sn, off) in SEG:
        for t0 in range(st0, st0 + sn, 512):
            n = min(512, st0 + sn - t0)
            h_, q_ = hcs[bi % 2], sqs[bi % 2]
            bi += 1
            bm, bq = k.banks[4], k.banks[5]
            for ct in range(2):
                bk = k.banks[ct]
                base = off + (t0 - st0) - 15
                for j in range(31):
                    P.op('pe', lambda e, bk=bk, ct=ct, j=j, base=base, n=n: e.matmul(
                        bk[:, 0:n], lhsT=DW[:, ct, j, :], rhs=GS[:, ct, base + j:base + j + n],
                        start=(j == 0), stop=(j == 30)), reads=[DW, GS], writes=[bk])
                P.op('act', lambda e, bk=bk, ct=ct, h_=h_, n=n: e.activation(
                    out=h_[:, ct, 0:n], in_=bk[:, 0:n], func=AF.Identity, bias=cvp[:, ct, 0:1]),
                    reads=[bk, cvp], writes=[h_])
                P.op('act', lambda e, bk=bk, ct=ct, q_=q_, n=n: e.activation(
                    out=q_[:, ct, 0:n], in_=bk[:, 0:n], func=AF.Square, bias=cvp[:, ct, 0:1]),
                    reads=[bk, cvp], writes=[q_])
            for ct in range(2):
                P.op('pe', lambda e, ct=ct, h_=h_, n=n: e.matmul(bm[:, 0:n], lhsT=onesM[:], rhs=h_[:, ct, 0:n],
                                                                 start=(ct == 0), stop=(ct == 1)),
                     reads=[onesM, h_], writes=[bm])
            for ct in range(2):
                P.op('pe', lambda e, ct=ct, q_=q_, n=n: e.matmul(bq[:, 0:n], lhsT=onesM[:], rhs=q_[:, ct, 0:n],
                                                                 start=(ct == 0), stop=(ct == 1)),
                     reads=[onesM, q_], writes=[bq])
            P.op('act', lambda e, n=n: e.activation(out=m2[:, 0:n], in_=bm[:, 0:n], func=AF.Square),
                 reads=[bm], writes=[m2])
            P.op('dve', lambda e, n=n: e.tensor_tensor(out=var[:, 0:n], in0=bq[:, 0:n], in1=m2[:, 0:n], op=ALU.subtract),
                 reads=[bq, m2], writes=[var])
            P.op('act', lambda e, n=n: e.activation(out=var[:, 0:n], in_=var[:, 0:n], func=AF.Ln, bias=epsln[:]),
                 reads=[var, epsln], writes=[var])
            P.op('act', lambda e, n=n: e.activation(out=var[:, 0:n], in_=var[:, 0:n], func=AF.Exp, scale=-0.5),
                 reads=[var], writes=[var])
            for ct in range(2):
                d_ = dd[ct]
                s_ = stg[si % 3]
                si += 1
                P.op('dve', lambda e, ct=ct, h_=h_, d_=d_, n=n: e.tensor_tensor(
                    out=d_[:, 0:n], in0=h_[:, ct, 0:n], in1=bm[:, 0:n], op=ALU.subtract),
                    reads=[h_, bm], writes=[d_])
                P.op('pool', lambda e, d_=d_, n=n: e.tensor_tensor(out=d_[:, 0:n], in0=d_[:, 0:n], in1=var[:, 0:n],
                                                                   op=ALU.mult), reads=[d_, var], writes=[d_])
                P.op('act', lambda e, ct=ct, d_=d_, s_=s_, n=n: e.activation(
                    out=s_[:, 0:n], in_=d_[:, 0:n], func=AF.Silu, scale=cvp[:, ct, 1:2], bias=cvp[:, ct, 2:3]),
                    reads=[d_, cvp], writes=[s_])
                P.dma('sp', mixT[768 + ct * 128:768 + (ct + 1) * 128, t0:t0 + n], s_[:, 0:n], reads=[s_],
                      writes=[P.dkey('mixT', 6 + ct, t0)])


def na_cfgs():
    cfgs = [(2, b) for b in range(0, 5)]
    win = {}
    for a in range(32):
        if a in (0, 1, 30, 31):
            blo = 0 if a < 2 else 28
            lst = []
            for b in range(blo, blo + 4):
                lst.append((b, len(cfgs)))
                cfgs.append((a, b))
            win[a] = lst
        else:
            win[a] = [(a + d, d + 2) for d in range(-2, 3)]
    return cfgs, win


def na_btab_host(rpb):
    cfgs, _ = na_cfgs()
    out = np.full((8, len(cfgs), 128, 128), NEG, np.float32)
    p = np.arange(128)
    kr, kc = p // 64, p % 64
    qr, qc = p // 64, p % 64
    for ci, (a, b) in enumerate(cfgs):
        i = 2 * a + qr[None, :]
        krow = 2 * b + kr[:, None]
        rs = np.clip(i - 4, 0, 56)
        vrow = (krow >= rs) & (krow < rs + 8)
        ws = np.clip(qc[None, :] - 8, 0, 48)
        vcol = (kc[:, None] >= ws) & (kc[:, None] < ws + 16)
        ri = np.clip(krow - i + 7, 0, 14)
        cidx = np.clip(kc[:, None] - qc[None, :] + 15, 0, 30)
        valid = vrow & vcol
        for h in range(8):
            g = rpb[h][ri, cidx]
            out[h, ci] = np.where(valid, g, NEG)
    return out


def phase_d(k, l, ph, with_ctx_q):
    P, ins = k.P, k.ins
    qkT = k.dram("qkT", [1024, T], BF16)
    vtm = k.dram("vtm", [T, 512], BF16)
    mixT = k.dram("mixT", [1024, T], BF16)
    cfgs, win = na_cfgs()
    NC_ = len(cfgs)
    QTs = P.sb([128, T], BF16, stack=ph)
    KTm = [P.sb([128, T], BF16, stack=ph) for _ in range(2)]
    Vaug = P.sb([128, NT, 2, 65], BF16, stack=ph)
    BT = P.sb([128, 2, NC_, 128], BF16, stack=ph)
    btstg = [P.sb([128, NC_, 128], F32, stack=ph) for _ in range(2)]
    ident8 = P.sb([128, 128], BF16, stack=ph)
    P.op('dve', lambda e: e.tensor_scalar(out=ident8[:], in0=k.ident[:], scalar1=8.0, scalar2=None, op0=ALU.mult),
         reads=[k.ident], writes=[ident8])
    P.op('pool', lambda e: e.memset(KTm[0][64:128, :], 0.0), writes=[KTm[0]])
    P.op('pool', lambda e: e.memset(KTm[1][0:64, :], 0.0), writes=[KTm[1]])
    P.op('pool', lambda e: e.memset(Vaug[:, :, :, 64:65], 1.0), writes=[Vaug])
    PT = [P.sb([128, 7, 128], BF16, stack=ph) for _ in range(2)]
    rs = [P.sb([128, 2], F32, stack=ph) for _ in range(2)]
    On = [P.sb([128, 128], F32, stack=ph) for _ in range(2)]
    stg = [P.sb([128, 512], BF16, stack=ph) for _ in range(2)]
    qk_keys = lambda ft: [b for kk, b in P.dk.items() if kk[0] == 'qkT' and kk[1] == ft]
    v_keys = [b for kk, b in P.dk.items() if kk[0] == 'vtm']
    qtiles = ([0, 1] if with_ctx_q else []) + list(range(2, NT))
    it = 0
    for ft in range(4):
        P.dma('sp', QTs[:], qkT[ft * 128:(ft + 1) * 128, :], reads=qk_keys(ft), writes=[QTs])
        for par in range(2):
            r0 = 512 + ft * 128 + par * 64
            P.dma('sp', KTm[par][par * 64:(par + 1) * 64, :], qkT[r0:r0 + 64, :], reads=qk_keys(4 + ft),
                  writes=[KTm[par]])
        vsrc = vtm.rearrange("(i p) (f hh d) -> p i f hh d", p=128, hh=2, d=64)
        for hh in range(2):
            for i0 in range(0, NT, 9):
                i1 = min(NT, i0 + 9)
                P.dma('sp', Vaug[:, i0:i1, hh, 0:64], vsrc[:, i0:i1, ft, hh, :], reads=v_keys, writes=[Vaug])
            bs_ = btstg[hh]
            P.dma('sp', bs_[:], ins['na_btab'][l][2 * ft + hh].rearrange("c k q -> k c q"), writes=[bs_])
            if hh == 0:
                P.op('dve', lambda e, bs_=bs_, hh=hh: e.tensor_copy(out=BT[:, hh, :, :], in_=bs_[:]), reads=[bs_], writes=[BT])
            else:
                P.op('act', lambda e, bs_=bs_, hh=hh: e.activation(out=BT[:, hh, :, :], in_=bs_[:], func=AF.Copy),
                     reads=[bs_], writes=[BT])
        sgi = [0]
        units = [(qi, qt, hh) for qi, qt in enumerate(qtiles) for hh in range(2)]

        def slots_of(qt):
            wl = [] if qt < 2 else win[qt - 2]
            return [(0, None), (1, None)] + [(2 + b, cfg) for (b, cfg) in wl]

        def s_exp(ui):
            qi, qt, hh = units[ui]
            slots = slots_of(qt)
            ns = len(slots)
            bS = [k.banks[(ui % 2) * 2], k.banks[(ui % 2) * 2 + 1]]
            pt = PT[ui % 2]
            for s_, (kt_, cfg) in enumerate(slots):
                bk = bS[s_ // 4]
                o0 = (s_ % 4) * 128
                P.op('pe', lambda e, bk=bk, o0=o0, hh=hh, kt_=kt_, qt=qt, s_=s_: e.matmul(
                    bk[:, o0:o0 + 128], lhsT=KTm[hh][:, kt_ * 128:(kt_ + 1) * 128],
                    rhs=QTs[:, qt * 128:(qt + 1) * 128], start=(s_ % 4 == 0), stop=True, skip_group_check=True),
                    reads=[KTm[hh], QTs], writes=[bk])
                if cfg is not None:
                    P.op('pe', lambda e, bk=bk, o0=o0, hh=hh, cfg=cfg: e.matmul(
                        bk[:, o0:o0 + 128], lhsT=ident8[:], rhs=BT[:, hh, cfg, :], start=False, stop=True,
                        skip_group_check=True), reads=[ident8, BT], writes=[bk])
            n0 = min(ns, 4)
            P.op('act', lambda e, pt=pt, b0=bS[0], n0=n0: e.activation(
                out=pt[:, 0:n0, :], in_=b0[:, 0:n0 * 128].rearrange("p (s q) -> p s q", q=128), func=AF.Exp,
                scale=0.125), reads=[bS[0]], writes=[pt])
            if ns > 4:
                P.op('act', lambda e, pt=pt, b1=bS[1], ns=ns: e.activation(
                    out=pt[:, 4:ns, :], in_=b1[:, 0:(ns - 4) * 128].rearrange("p (s q) -> p s q", q=128),
                    func=AF.Exp, scale=0.125), reads=[bS[1]], writes=[pt])

        def pv(ui):
            qi, qt, hh = units[ui]
            slots = slots_of(qt)
            pt = PT[ui % 2]
            bO = k.banks[4 + qi % 2]
            for s_, (kt_, cfg) in enumerate(slots):
                P.op('pe', lambda e, bO=bO, hh=hh, pt=pt, s_=s_, kt_=kt_: e.matmul(
                    bO[:, hh * 65:(hh + 1) * 65], lhsT=pt[:, s_, :], rhs=Vaug[:, kt_, hh, :],
                    start=(hh == 0 and s_ == 0), stop=True, skip_group_check=True),
                    reads=[pt, Vaug], writes=[bO])
            if hh == 1:
                fin(qi, qt, bO)

        def fin(qi, qt, bO):
            r_, o_ = rs[qi % 2], On[qi % 2]
            bOv = bO[:, 0:130].rearrange("p (h d) -> p h d", d=65)
            P.op('dve', lambda e, r_=r_, bOv=bOv: e.reciprocal(out=r_[:], in_=bOv[:, :, 64]), reads=[bO], writes=[r_])
            P.op('dve', lambda e, r_=r_, o_=o_, bOv=bOv: e.tensor_tensor(
                out=o_[:].rearrange("p (h d) -> p h d", d=64), in0=bOv[:, :, 0:64],
                in1=r_[:].unsqueeze(2).broadcast_to([128, 2, 64]), op=ALU.mult), reads=[bO, r_], writes=[o_])
            bT = k.banks[6 + qi % 2]
            P.op('pe', lambda e, bT=bT, o_=o_: e.transpose(out=bT[:, 0:128], in_=o_[:], identity=k.ident[:]),
                 reads=[o_, k.ident], writes=[bT])
            st_ = stg[sgi[0] % 2]
            slot = qi % 4
            P.op('act', lambda e, bT=bT, st_=st_, slot=slot: e.activation(
                out=st_[:, slot * 128:(slot + 1) * 128], in_=bT[:, 0:128], func=AF.Copy), reads=[bT], writes=[st_])
            last = (qi == len(qtiles) - 1)
            if slot == 3 or last or qtiles[qi + 1] != qt + 1:
                q0 = qt - slot
                P.dma('sp', mixT[ft * 128:(ft + 1) * 128, q0 * 128:(qt + 1) * 128], st_[:, 0:(slot + 1) * 128],
                      reads=[st_], writes=[P.dkey('mixT', ft, q0)])
                sgi[0] += 1

        s_exp(0)
        for ui in range(len(units)):
            if ui + 1 < len(units):
                s_exp(ui + 1)
            pv(ui)


CH = 128
LEV = CH.bit_length() - 2
NCTX = LC // CH
NCH = T // CH
GN_EPS = 64e-5
DECAY_C = 0.6065306597126334


def phase_e(k, l, ph, first_tok=0, dbg_n=None, dbg_stage=99, dbg_dirs=2):
    P, ins = k.P, k.ins
    utm = k.dram("utm", [T, RW_IN], F32)
    rwo = k.dram("rwo", [2, T, 256], F32)
    mixT = k.dram("mixT", [1024, T], BF16)
    bctr = [0]

    def nb():
        b = k.banks[bctr[0] % 8]
        bctr[0] += 1
        return b

    sb = lambda shape, dt=F32: P.sb(shape, dt, stack=ph)
    rep = lambda src, n=CH: src.partition_broadcast(n)
    W0rep, A0rep = sb([CH, 2, 256]), sb([CH, 2, 256])
    for d in range(2):
        P.dma('sp', W0rep[:, d, :], rep(ins['rw_w0'][l][d]), writes=[W0rep])
        P.dma('sp', A0rep[:, d, :], rep(ins['rw_a0'][l][d]), writes=[A0rep])
    KKrep, KArep, OMKA, RKrep, GNG, GNB = [sb([CH, 256]) for _ in range(6)]
    P.dma('sp', KKrep[:], rep(ins['rw_k_k'][l]), writes=[KKrep])
    P.dma('sp', KArep[:], rep(ins['rw_k_a'][l]), writes=[KArep])
    P.dma('sp', RKrep[:], rep(ins['rw_r_k'][l].rearrange("h d -> (h d)")), writes=[RKrep])
    P.dma('sp', GNG[:], rep(ins['rw_gn_g'][l]), writes=[GNG])
    P.dma('sp', GNB[:], rep(ins['rw_gn_b'][l]), writes=[GNB])
    P.op('dve', lambda e: e.tensor_scalar(out=OMKA[:], in0=KArep[:], scalar1=-1.0, scalar2=1.0, op0=ALU.mult,
                                          op1=ALU.add), reads=[KArep], writes=[OMKA])
    W2p, A2p, G2 = sb([128, 2, 256]), sb([128, 2, 256]), sb([128, 2, 256])
    P.op('pool', lambda e: e.memset(W2p[:], 0.0), writes=[W2p])
    P.op('pool', lambda e: e.memset(A2p[:], 0.0), writes=[A2p])
    for d in range(2):
        P.dma('sp', W2p[0:64, d, :], ins['rw_w2'][l][d], writes=[W2p])
        P.dma('sp', A2p[64:128, d, :], ins['rw_a2'][l][d], writes=[A2p])
        P.dma('sp', G2[:, d, :], ins['rw_g2'][l][d], writes=[G2])
    eps12, epsgn = sb([CH, 1]), sb([CH, 1])
    P.op('pool', lambda e: e.memset(eps12[:], 1e-12), writes=[eps12])
    P.op('pool', lambda e: e.memset(epsgn[:], GN_EPS), writes=[epsgn])
    ones64 = sb([CH, 128])
    P.op('pool', lambda e: e.memset(ones64[:], 1.0), writes=[ones64])
    Mst, Min = [sb([CH, 4, CH]) for _ in range(2)], [sb([CH, 4, CH]) for _ in range(2)]
    MstT = [sb([CH, 4, CH]) for _ in range(2)]
    for d in range(2):
        sgn = 1 if d == 0 else -1
        for (m, op, tr) in ((Mst[d], ALU.is_gt, 1), (Min[d], ALU.is_ge, 1), (MstT[d], ALU.is_gt, -1)):
            P.op('pool', lambda e, m=m: e.memset(m[:], 1.0), writes=[m])
            P.op('pool', lambda e, m=m, op=op, sg=sgn * tr: e.affine_select(
                out=m[:], in_=m[:], pattern=[[0, 4], [sg, CH]], compare_op=op, fill=0.0, base=0,
                channel_multiplier=-sg), reads=[m], writes=[m])
    MD, MDT, MET = Mst, MstT, None
    if CH == 128:
        SB = sb([CH, 4, CH])
        P.op('pool', lambda e: e.memset(SB[:], 0.0), writes=[SB])
        P.op('pool', lambda e: e.memset(SB[0:64, :, 0:64], 1.0), writes=[SB])
        P.op('pool', lambda e: e.memset(SB[64:128, :, 64:128], 1.0), writes=[SB])
        MD, MDT, MET = [sb([CH, 4, CH]) for _ in range(2)], [sb([CH, 4, CH]) for _ in range(2)], [sb([CH, 4, CH]) for _ in range(2)]
        for d in range(2):
            P.op('dve', lambda e, d=d: e.tensor_tensor(out=MD[d][:], in0=Mst[d][:], in1=SB[:], op=ALU.mult),
                 reads=[Mst[d], SB], writes=[MD[d]])
            P.op('dve', lambda e, d=d: e.tensor_tensor(out=MDT[d][:], in0=MstT[d][:], in1=SB[:], op=ALU.mult),
                 reads=[MstT[d], SB], writes=[MDT[d]])
            P.op('dve', lambda e, d=d: e.tensor_tensor(out=MET[d][:], in0=MstT[d][:], in1=MDT[d][:], op=ALU.subtract),
                 reads=[MstT[d], MDT[d]], writes=[MET[d]])
    I64 = sb([CH, 4, CH])
    P.op('dve', lambda e: e.tensor_copy(out=I64[:], in_=k.ident[0:CH, 0:CH].unsqueeze(1).broadcast_to([CH, 4, CH])),
         reads=[k.ident], writes=[I64])
    bmask = sb([128, 2, 2, 64])
    P.op('pool', lambda e: e.memset(bmask[:], 0.0), writes=[bmask])
    P.op('pool', lambda e: e.memset(bmask[0:64, :, 0, :], 1.0), writes=[bmask])
    P.op('pool', lambda e: e.memset(bmask[64:128, :, 1, :], 1.0), writes=[bmask])
    def alloc_work():
        Uc = [sb([CH, RW_IN]) for _ in range(2)]
        LT = sb([128, 2, CH])
        tmpA, tmpB = sb([CH, 256]), sb([CH, 256])
        LW, Aa, Gs = sb([CH, 256]), sb([CH, 256]), [sb([CH, 256]) for _ in range(2)]
        kkr, sq, kk, zz = sb([CH, 256]), sb([CH, 256]), sb([CH, 256]), sb([CH, 256])
        s4, rn = sb([CH, 4]), sb([CH, 4])
        kd, bb, tt = sb([CH, 256]), sb([CH, 256]), sb([CH, 256])
        EC, EZ, EN, ET, EH = [sb([CH, 256]) for _ in range(5)]
        RT, ZT, BN, KN = [sb([CH, 256]) for _ in range(4)]
        BH, KH, Vb = ([sb([CH, 256], BF16) for _ in range(2)] for _ in range(3))
        PL1, PL2 = [sb([128, 4, CH], BF16) for _ in range(2)], sb([128, 4, CH], BF16)
        EX1 = sb([128, 4, 2, CH], BF16)
        EX2 = sb([128, 4, 2, CH], BF16)
        P.op('pool', lambda e: e.memset(EX1[:], 0.0), writes=[EX1])
        P.op('pool', lambda e: e.memset(EX2[:], 0.0), writes=[EX2])
        Xm, Ym = [sb([CH, 4, CH], BF16) for _ in range(2)], [sb([CH, 4, CH], BF16) for _ in range(2)]
        Tt = [sb([CH, 4, CH], BF16) for _ in range(2)]
        Ttt = [sb([CH, 4, CH], BF16) for _ in range(2)]
        YE, Asb = sb([CH, 4, CH], BF16), sb([CH, 4, CH], BF16)
        Azk, Arb, Ark = ([sb([CH, 4, CH], BF16) for _ in range(2)] for _ in range(3))
        TF = [sb([CH, 4, CH], BF16) for _ in range(2)]
        Wsb, Usb = sb([CH, 256], BF16), sb([CH, 256], BF16)
        Gam = [sb([128, 2, 128]) for _ in range(2)]
        HE = sb([128, 2, 2, 64])
        HEb = sb([128, 2, 2, 64], BF16)
        htmp = sb([128, 2, 2, 64])
        ysq, yn, rk, oo, prev = [sb([CH, 256]) for _ in range(5)]
        bon = [sb([CH, 256]) for _ in range(2)]
        st1, st2, mean, var_ = sb([CH, 4]), sb([CH, 4]), sb([CH, 4]), sb([CH, 4])
        bsum = sb([CH, 4])
        ostg = [sb([128, 2, CH], BF16) for _ in range(2)]

        return dict(Uc=Uc, LT=LT, tmpA=tmpA, tmpB=tmpB, LW=LW, Aa=Aa, Gs=Gs, kkr=kkr, sq=sq, kk=kk, zz=zz, s4=s4, rn=rn, kd=kd, bb=bb, tt=tt, EC=EC, EZ=EZ, EN=EN, ET=ET, EH=EH, RT=RT, ZT=ZT, BN=BN, KN=KN, BH=BH, KH=KH, Vb=Vb, PL1=PL1, PL2=PL2, EX1=EX1, EX2=EX2, Xm=Xm, Ym=Ym, Tt=Tt, Ttt=Ttt, YE=YE, Asb=Asb, Azk=Azk, Arb=Arb, Ark=Ark, TF=TF, Wsb=Wsb, Usb=Usb, Gam=Gam, HE=HE, HEb=HEb, htmp=htmp, ysq=ysq, yn=yn, rk=rk, bon=bon, oo=oo, prev=prev, st1=st1, st2=st2, mean=mean, var_=var_, bsum=bsum, ostg=ostg)

    v4 = lambda t: t[:].rearrange("p (h d) -> p h d", d=64)
    bc4 = lambda t: t[:].unsqueeze(2).broadcast_to([CH, 4, 64])
    TT = lambda eng, out, a, b, op, rd, wr: P.op(eng, lambda e: e.tensor_tensor(out=out, in0=a, in1=b, op=op),
                                                 reads=rd, writes=wr)
    ACT = lambda out, in_, func, rd, wr, **kw: P.op('act', lambda e: e.activation(out=out, in_=in_, func=func, **kw),
                                                    reads=rd, writes=wr)

    ident64 = k.ident[0:CH, 0:CH]

    def TR(bk, out, in_, rd):
        P.op('pe', lambda e: e.transpose(out=out, in_=in_, identity=ident64), reads=list(rd) + [k.ident], writes=[bk])

    def CP(eng, out, in_, rd, wr):
        if eng == 'act':
            P.op('act', lambda e: e.activation(out=out, in_=in_, func=AF.Copy), reads=rd, writes=wr)
        else:
            P.op(eng, lambda e: e.tensor_copy(out=out, in_=in_), reads=rd, writes=wr)

    def TS(eng, out, in0, s1, s2, op0, op1, rd, wr):
        if op1 is None:
            P.op(eng, lambda e: e.tensor_scalar(out=out, in0=in0, scalar1=s1, scalar2=None, op0=op0), reads=rd, writes=wr)
        else:
            P.op(eng, lambda e: e.tensor_scalar(out=out, in0=in0, scalar1=s1, scalar2=s2, op0=op0, op1=op1),
                 reads=rd, writes=wr)

    def RED(out, in_, rd, wr):
        P.op('dve', lambda e: e.tensor_reduce(out=out, in_=in_, axis=AX.X, op=ALU.add), reads=rd, writes=wr)

    def mm(bk, out, lhsT, rhs, start, rd):
        P.op('pe', lambda e: e.matmul(out, lhsT=lhsT, rhs=rhs, start=start, stop=True, skip_group_check=True),
             reads=rd, writes=[bk])

    def pre(d, n, c, W):
        Uc, LT, tmpA, tmpB, LW, Aa, Gs, kkr, sq, kk, zz, s4, rn, kd, bb, tt, EC, EZ, EN, ET, EH, RT, ZT, BN, KN, BH, KH, Vb, PL1, PL2, EX1, EX2, Xm, Ym, Tt, Ttt, YE, Asb, Azk, Arb, Ark, TF, Wsb, Usb, Gam, HE, HEb, htmp, ysq, yn, rk, bon, oo, prev, st1, st2, mean, var_, bsum, ostg = W['Uc'], W['LT'], W['tmpA'], W['tmpB'], W['LW'], W['Aa'], W['Gs'], W['kkr'], W['sq'], W['kk'], W['zz'], W['s4'], W['rn'], W['kd'], W['bb'], W['tt'], W['EC'], W['EZ'], W['EN'], W['ET'], W['EH'], W['RT'], W['ZT'], W['BN'], W['KN'], W['BH'], W['KH'], W['Vb'], W['PL1'], W['PL2'], W['EX1'], W['EX2'], W['Xm'], W['Ym'], W['Tt'], W['Ttt'], W['YE'], W['Asb'], W['Azk'], W['Arb'], W['Ark'], W['TF'], W['Wsb'], W['Usb'], W['Gam'], W['HE'], W['HEb'], W['htmp'], W['ysq'], W['yn'], W['rk'], W['bon'], W['oo'], W['prev'], W['st1'], W['st2'], W['mean'], W['var_'], W['bsum'], W['ostg']
        nb = W["nb_pre"]
        Azk, Arb, Ark, PL1, BH, KH, Vb, Gam, Gs, bon, TF = Azk[n % 2], Arb[n % 2], Ark[n % 2], PL1[n % 2], BH[n % 2], KH[n % 2], Vb[n % 2], Gam[n % 2], Gs[n % 2], bon[n % 2], TF[n % 2]
        pass
        t0 = c * CH
        U_ = Uc[n % 2]
        P.dma('sp', U_[:], utm[t0:t0 + CH, :], reads=[P.dkey('utm', t0 // 128)], writes=[U_])
        r_, kraw, v_ = U_[:, 0:256], U_[:, 256:512], U_[:, 512:768]
        lo = 768 + d * 256
        bL = nb()
        for j in range(2):
            TR(bL, bL[:, j * CH:(j + 1) * CH], U_[:, lo + j * 128:lo + (j + 1) * 128], [U_])
        ACT(LT[0:64, 0, :], bL[0:64, 0:CH], AF.Tanh, [bL], [LT])
        ACT(LT[64:128, 0, :], bL[64:128, 0:CH], AF.Copy, [bL], [LT])
        ACT(LT[:, 1, :], bL[:, CH:2 * CH], AF.Sigmoid, [bL], [LT])
        bX = nb()
        mm(bX, bX[0:CH, 0:256], LT[:, 0, :], W2p[:, d, :], True, [LT, W2p])
        mm(bX, bX[0:CH, 256:512], LT[:, 0, :], A2p[:, d, :], True, [LT, A2p])
        bG = nb()
        mm(bG, bG[0:CH, 0:256], LT[:, 1, :], G2[:, d, :], True, [LT, G2])
        TT('dve', tmpA[:], bX[0:CH, 0:256], W0rep[:, d, :], ALU.add, [bX, W0rep], [tmpA])
        ACT(tmpA[:], tmpA[:], AF.Sigmoid, [tmpA], [tmpA])
        ACT(LW[:], tmpA[:], AF.Copy, [tmpA], [LW], scale=-DECAY_C)
        TT('dve', tmpB[:], bX[0:CH, 256:512], A0rep[:, d, :], ALU.add, [bX, A0rep], [tmpB])
        ACT(Aa[:], tmpB[:], AF.Sigmoid, [tmpB], [Aa])
        ACT(Gs[:], bG[0:CH, 0:256], AF.Copy, [bG], [Gs])
        yield
        TT('dve', kkr[:], kraw, KKrep[:], ALU.mult, [U_, KKrep], [kkr])
        ACT(sq[:], kkr[:], AF.Square, [kkr], [sq])
        RED(s4[:], v4(sq), [sq], [s4])
        ACT(rn[:], s4[:], AF.Ln, [s4, eps12], [rn], bias=eps12[:])
        ACT(rn[:], rn[:], AF.Exp, [rn], [rn], scale=-0.5)
        TT('dve', v4(kk), v4(kkr), bc4(rn), ALU.mult, [kkr, rn], [kk])
        ACT(zz[:], kk[:], AF.Copy, [kk], [zz], scale=-1.0)
        TT('dve', tt[:], Aa[:], KArep[:], ALU.mult, [Aa, KArep], [tt])
        TT('dve', tt[:], tt[:], OMKA[:], ALU.add, [tt, OMKA], [tt])
        TT('dve', kd[:], kraw, tt[:], ALU.mult, [U_, tt], [kd])
        TT('dve', bb[:], kk[:], Aa[:], ALU.mult, [kk, Aa], [bb])
        yield
        bC = nb()
        mm(bC, bC[0:CH, 0:256], Min[d][:, 0, :], LW[:], True, [Min[d], LW])
        mm(bC, bC[0:CH, 256:512], ones64[:, 0:CH], LW[:], True, [ones64, LW])
        yield
        ACT(EC[:], bC[0:CH, 0:256], AF.Exp, [bC], [EC])
        ACT(EN[:], bC[0:CH, 0:256], AF.Exp, [bC], [EN], scale=-1.0)
        ACT(ET[:], bC[0:CH, 256:512], AF.Exp, [bC], [ET])
        TT('dve', EZ[:], bC[0:CH, 0:256], LW[:], ALU.subtract, [bC, LW], [EZ])
        ACT(EZ[:], EZ[:], AF.Exp, [EZ], [EZ])
        yield
        TT('dve', EH[:], ET[:], EN[:], ALU.mult, [ET, EN], [EH])
        TT('dve', RT[:], r_, EC[:], ALU.mult, [U_, EC], [RT])
        TT('dve', ZT[:], zz[:], EZ[:], ALU.mult, [zz, EZ], [ZT])
        TT('dve', BN[:], bb[:], EN[:], ALU.mult, [bb, EN], [BN])
        TT('dve', KN[:], kd[:], EN[:], ALU.mult, [kd, EN], [KN])
        yield
        TT('dve', BH[:], bb[:], EH[:], ALU.mult, [bb, EH], [BH])
        yield
        TT('dve', KH[:], kd[:], EH[:], ALU.mult, [kd, EH], [KH])
        yield
        ACT(Vb[:], U_[:, 512:768], AF.Copy, [U_], [Vb])
        yield
        bT1, bT2 = nb(), nb()
        for j, src in enumerate((RT, ZT)):
            for pr in range(2):
                o0 = (j * 2 + pr) * CH
                TR(bT1, bT1[:, o0:o0 + CH], src[:, pr * 128:(pr + 1) * 128], [src])
        for j, src in enumerate((BN, KN)):
            for pr in range(2):
                o0 = (j * 2 + pr) * CH
                TR(bT2, bT2[:, o0:o0 + CH], src[:, pr * 128:(pr + 1) * 128], [src])
        b1v = lambda p0, p1: bT1[p0:p1, 0:4 * CH].rearrange("p (j t) -> p j t", t=CH)
        b2v = lambda p0, p1: bT2[p0:p1, 0:4 * CH].rearrange("p (j t) -> p j t", t=CH)
        yield
        ACT(PL1[:], b1v(0, 128), AF.Copy, [bT1], [PL1])
        CP('act', PL2[:], b2v(0, 128), [bT2], [PL2])
        yield
        ACT(EX1[0:64, :, 0, :], b1v(0, 64), AF.Copy, [bT1], [EX1])
        CP('dve', EX1[64:128, :, 1, :], b1v(64, 128), [bT1], [EX1])
        yield
        ACT(EX2[0:64, :, 0, :], b2v(0, 64), AF.Copy, [bT2], [EX2])
        yield
        CP('act', EX2[64:128, :, 1, :], b2v(64, 128), [bT2], [EX2])
        yield
        ex = lambda E_, j: E_[:, j, :, :].rearrange("p a t -> p (a t)")
        f4 = lambda t: t[:].rearrange("p h t -> p (h t)")
        prods = [(PL2, 0, EX1, 2, [(Xm[0], MD[d])]),
                 (PL1, 2, EX2, 0, [(Ym[0], MDT[d])] + ([(YE, MET[d])] if CH == 128 else [])),
                 (PL2, 2, EX1, 2, [(Azk, Mst[d])]),
                 (PL2, 0, EX1, 0, [(Arb, Min[d])]),
                 (PL2, 2, EX1, 0, [(Ark, Min[d])])]
        for (L_, lo_, E_, eo_, outs) in prods:
            bA = nb()
            for pr in range(2):
                cs = slice(pr * 2 * CH, (pr + 1) * 2 * CH)
                mm(bA, bA[0:CH, cs], L_[:, lo_ + pr, :], ex(E_, eo_ + pr), pr == 0, [L_, E_])
            for (dst, msk) in outs:
                TT('dve', f4(dst), bA[0:CH, 0:4 * CH], f4(msk), ALU.mult, [bA, msk], [dst])
        yield
        if CH == 128:
            TT('pool', f4(Tt[0]), f4(Xm[0]), f4(I64), ALU.add, [Xm[0], I64], [Tt[0]])
            TT('dve', f4(Ttt[0]), f4(Ym[0]), f4(I64), ALU.add, [Ym[0], I64], [Ttt[0]])
            for lev in range(5):
                Xc, Yc, Tc, Uc_ = Xm[lev % 2], Ym[lev % 2], Tt[lev % 2], Ttt[lev % 2]
                Xn, Yn, Tn, Un = Xm[(lev + 1) % 2], Ym[(lev + 1) % 2], Tt[(lev + 1) % 2], Ttt[(lev + 1) % 2]
                bY = nb()
                for h in range(4):
                    mm(bY, bY[0:CH, h * CH:(h + 1) * CH], Xc[:, h, :], Yc[:, h, :], h == 0, [Xc, Yc])
                bXx = nb()
                for h in range(4):
                    mm(bXx, bXx[0:CH, h * CH:(h + 1) * CH], Yc[:, h, :], Xc[:, h, :], h == 0, [Xc, Yc])
                ACT(f4(Xn), bXx[0:CH, 0:4 * CH], AF.Copy, [bXx], [Xn])
                CP('act', f4(Yn), bY[0:CH, 0:4 * CH], [bY], [Yn])
                yield
                bTt = nb()
                for h in range(4):
                    mm(bTt, bTt[0:CH, h * CH:(h + 1) * CH], Yn[:, h, :], Tc[:, h, :], h == 0, [Yn, Tc])
                bTu = nb()
                for h in range(4):
                    mm(bTu, bTu[0:CH, h * CH:(h + 1) * CH], Xn[:, h, :], Uc_[:, h, :], h == 0, [Xn, Uc_])
                TT('dve', f4(Tn), bTt[0:CH, 0:4 * CH], f4(Tc), ALU.add, [bTt, Tc], [Tn])
                TT('dve', f4(Un), bTu[0:CH, 0:4 * CH], f4(Uc_), ALU.add, [bTu, Uc_], [Un])
                yield
            TD, TDt = Tt[1], Ttt[1]
            bA1 = nb()
            for h in range(4):
                mm(bA1, bA1[0:CH, h * CH:(h + 1) * CH], YE[:, h, :], TD[:, h, :], h == 0, [YE, TD])
            ACT(f4(Asb), bA1[0:CH, 0:4 * CH], AF.Copy, [bA1], [Asb])
            bZ = nb()
            for h in range(4):
                mm(bZ, bZ[0:CH, h * CH:(h + 1) * CH], TDt[:, h, :], Asb[:, h, :], h == 0, [TDt, Asb])
            TT('dve', f4(TF), bZ[0:CH, 0:4 * CH], f4(TD), ALU.add, [bZ, TD], [TF])
            yield
        else:
            TT('pool', f4(Tt[0]), f4(Xm[0]), f4(I64), ALU.add, [Xm[0], I64], [Tt[0]])
            for lev in range(LEV):
                Xc, Yc, Tc = Xm[lev % 2], Ym[lev % 2], Tt[lev % 2]
                Xn, Yn, Tn = Xm[(lev + 1) % 2], Ym[(lev + 1) % 2], (Tt[(lev + 1) % 2] if lev < LEV - 1 else TF)
                bY = nb()
                for h in range(4):
                    mm(bY, bY[0:CH, h * CH:(h + 1) * CH], Xc[:, h, :], Yc[:, h, :], h == 0, [Xc, Yc])
                if lev < LEV - 1:
                    bXx = nb()
                    for h in range(4):
                        mm(bXx, bXx[0:CH, h * CH:(h + 1) * CH], Yc[:, h, :], Xc[:, h, :], h == 0, [Xc, Yc])
                    ACT(f4(Xn), bXx[0:CH, 0:4 * CH], AF.Copy, [bXx], [Xn])
                CP('act', f4(Yn), bY[0:CH, 0:4 * CH], [bY], [Yn])
                bTt = nb()
                for h in range(4):
                    mm(bTt, bTt[0:CH, h * CH:(h + 1) * CH], Yn[:, h, :], Tc[:, h, :], h == 0, [Yn, Tc])
                TT('dve', f4(Tn), bTt[0:CH, 0:4 * CH], f4(Tc), ALU.add, [bTt, Tc], [Tn])
                yield
            yield

        bGm = nb()
        for pr in range(2):
            mm(bGm, bGm[:, pr * 128:(pr + 1) * 128], LW[:, pr * 128:(pr + 1) * 128], ones64[:, :], pr == 0,
               [LW, ones64])
        ACT(Gam[:].rearrange("p a b -> p (a b)"), bGm[:, 0:256], AF.Exp, [bGm], [Gam])
        yield
        TT('dve', rk[:], r_, RKrep[:], ALU.mult, [U_, RKrep], [rk])
        TT('dve', rk[:], rk[:], kd[:], ALU.mult, [rk, kd], [rk])
        RED(bsum[:], v4(rk), [rk], [bsum])
        TT('dve', v4(bon), U_[:, 512:768].rearrange("p (h d) -> p h d", d=64), bc4(bsum), ALU.mult,
           [U_, bsum], [bon])
        yield

    def post(d, n, c, W):
        Uc, LT, tmpA, tmpB, LW, Aa, Gs, kkr, sq, kk, zz, s4, rn, kd, bb, tt, EC, EZ, EN, ET, EH, RT, ZT, BN, KN, BH, KH, Vb, PL1, PL2, EX1, EX2, Xm, Ym, Tt, Ttt, YE, Asb, Azk, Arb, Ark, TF, Wsb, Usb, Gam, HE, HEb, htmp, ysq, yn, rk, bon, oo, prev, st1, st2, mean, var_, bsum, ostg = W['Uc'], W['LT'], W['tmpA'], W['tmpB'], W['LW'], W['Aa'], W['Gs'], W['kkr'], W['sq'], W['kk'], W['zz'], W['s4'], W['rn'], W['kd'], W['bb'], W['tt'], W['EC'], W['EZ'], W['EN'], W['ET'], W['EH'], W['RT'], W['ZT'], W['BN'], W['KN'], W['BH'], W['KH'], W['Vb'], W['PL1'], W['PL2'], W['EX1'], W['EX2'], W['Xm'], W['Ym'], W['Tt'], W['Ttt'], W['YE'], W['Asb'], W['Azk'], W['Arb'], W['Ark'], W['TF'], W['Wsb'], W['Usb'], W['Gam'], W['HE'], W['HEb'], W['htmp'], W['ysq'], W['yn'], W['rk'], W['bon'], W['oo'], W['prev'], W['st1'], W['st2'], W['mean'], W['var_'], W['bsum'], W['ostg']
        nb = W["nb_post"]
        Azk, Arb, Ark, PL1, BH, KH, Vb, Gam, Gs, bon, TF = Azk[n % 2], Arb[n % 2], Ark[n % 2], PL1[n % 2], BH[n % 2], KH[n % 2], Vb[n % 2], Gam[n % 2], Gs[n % 2], bon[n % 2], TF[n % 2]
        t0 = c * CH
        Tf = TF
        f4 = lambda t: t[:].rearrange("p h t -> p (h t)")
        he = lambda pr: HEb[:, pr, :, :].rearrange("p a t -> p (a t)")
        bW = nb()
        for h in range(4):
            mm(bW, bW[0:CH, h * 64:(h + 1) * 64], Azk[:, h, :], Vb[:, h * 64:(h + 1) * 64], h == 0, [Azk, Vb])
        for pr in range(2):
            mm(bW, bW[0:CH, pr * 128:(pr + 1) * 128], PL1[:, 2 + pr, :], he(pr), False, [PL1, HEb])
        ACT(Wsb[:], bW[0:CH, 0:256], AF.Copy, [bW], [Wsb])
        yield
        bU = nb()
        for h in range(4):
            mm(bU, bU[0:CH, h * 64:(h + 1) * 64], Tf[:, h, :], Wsb[:, h * 64:(h + 1) * 64], h == 0, [Tf, Wsb])
        CP('act', Usb[:], bU[0:CH, 0:256], [bU], [Usb])
        yield
        bYy = nb()
        for h in range(4):
            hs = slice(h * 64, (h + 1) * 64)
            mm(bYy, bYy[0:CH, hs], Arb[:, h, :], Usb[:, hs], h == 0, [Arb, Usb])
        for h in range(4):
            hs = slice(h * 64, (h + 1) * 64)
            mm(bYy, bYy[0:CH, hs], Ark[:, h, :], Vb[:, hs], False, [Ark, Vb])
        for pr in range(2):
            mm(bYy, bYy[0:CH, pr * 128:(pr + 1) * 128], PL1[:, pr, :], he(pr), False, [PL1, HEb])
        bH = nb()
        for pr in range(2):
            cs = slice(pr * 128, (pr + 1) * 128)
            mm(bH, bH[:, cs], BH[:, cs], Usb[:, cs], pr == 0, [BH, Usb])
        for pr in range(2):
            cs = slice(pr * 128, (pr + 1) * 128)
            mm(bH, bH[:, cs], KH[:, cs], Vb[:, cs], False, [KH, Vb])
        fl = lambda t: t[:].rearrange("p a b c -> p (a b c)")
        TT('dve', fl(htmp), bH[:, 0:256], fl(bmask), ALU.mult, [bH, bmask], [htmp])
        TT('dve', fl(HE), fl(HE), Gam[:].rearrange("p a b -> p (a b)"), ALU.mult, [HE, Gam], [HE])
        TT('dve', fl(HE), fl(HE), fl(htmp), ALU.add, [HE, htmp], [HE])
        ACT(fl(HEb), fl(HE), AF.Copy, [HE], [HEb])
        yield
        if t0 >= first_tok:
            yv = bYy[0:CH, 0:256].rearrange("p (h d) -> p h d", d=64)
            RED(st1[:], yv, [bYy], [st1])
            ACT(ysq[:], bYy[0:CH, 0:256], AF.Square, [bYy], [ysq])
            RED(st2[:], v4(ysq), [ysq], [st2])
            ACT(mean[:], st1[:], AF.Copy, [st1], [mean], scale=1.0 / 64)
            ACT(var_[:], mean[:], AF.Square, [mean], [var_])
            P.op('dve', lambda e: e.scalar_tensor_tensor(out=var_[:], in0=st2[:], scalar=1.0 / 64, in1=var_[:],
                                                         op0=ALU.mult, op1=ALU.subtract),
                 reads=[st2, var_], writes=[var_])
            ACT(var_[:], var_[:], AF.Ln, [var_, epsgn], [var_], bias=epsgn[:])
            ACT(var_[:], var_[:], AF.Exp, [var_], [var_], scale=-0.5)
            TT('dve', v4(yn), yv, bc4(mean), ALU.subtract, [bYy, mean], [yn])
            TT('dve', v4(yn), v4(yn), bc4(var_), ALU.mult, [yn, var_], [yn])
            TT('pool', yn[:], yn[:], GNG[:], ALU.mult, [yn, GNG], [yn])
            TT('pool', yn[:], yn[:], GNB[:], ALU.add, [yn, GNB], [yn])
            TT('pool', oo[:], yn[:], bon[:], ALU.add, [yn, bon], [oo])
            TT('dve', oo[:], oo[:], Gs[:], ALU.mult, [oo, Gs], [oo])
            n_other = (NCTX - 1 - c if c < NCTX else NCH - 1 - c + NCTX) if d == 0 else c
            if n_other > n:
                P.dma('pool', rwo[d, t0:t0 + CH, :], oo[:], reads=[oo], writes=[P.dkey('rwo', d, c)])
            else:
                P.dma('sp', prev[:], rwo[1 - d, t0:t0 + CH, :], reads=[P.dkey('rwo', 1 - d, c)], writes=[prev])
                TT('dve', oo[:], oo[:], prev[:], ALU.add, [oo, prev], [oo])
                bO = nb()
                for ct in range(2):
                    TR(bO, bO[:, ct * CH:(ct + 1) * CH], oo[:, ct * 128:(ct + 1) * 128], [oo])
                og = ostg[n % 2]
                ACT(og[:].rearrange("p a t -> p (a t)"), bO[:, 0:2 * CH], AF.Copy, [bO], [og])
                for ct in range(2):
                    P.dma('pool', mixT[512 + ct * 128:512 + (ct + 1) * 128, t0:t0 + CH], og[:, ct, :], reads=[og],
                          writes=[P.dkey('mixT', 4 + ct, t0)])
        yield

    Ws = [alloc_work(), alloc_work()]
    for d in range(2):
        for ci, nm in enumerate(("nb_pre", "nb_post")):
            ctr = [0]

            def nb_d(base=d * 4 + ci * 2, ctr=ctr):
                b = k.banks[base + ctr[0] % 2]
                ctr[0] += 1
                return b
            Ws[d][nm] = nb_d
        HE_, HEb_ = Ws[d]['HE'], Ws[d]['HEb']
        P.op('pool', lambda e, HE_=HE_: e.memset(HE_[:], 0.0), writes=[HE_])
        P.op('pool', lambda e, HEb_=HEb_: e.memset(HEb_[:], 0.0), writes=[HEb_])
    orders = [list(range(NCH)), list(range(NCTX - 1, -1, -1)) + list(range(NCH - 1, NCTX - 1, -1))]
    nsteps = dbg_n or NCH

    def run(gens):
        while gens:
            for g in list(gens):
                try:
                    next(g)
                except StopIteration:
                    gens.remove(g)

    run([pre(d, 0, orders[d][0], Ws[d]) for d in range(dbg_dirs)])
    for n in range(nsteps):
        gens = [post(d, n, orders[d][n], Ws[d]) for d in range(dbg_dirs)]
        if n + 1 < nsteps:
            gens += [pre(d, n + 1, orders[d][n + 1], Ws[d]) for d in range(dbg_dirs)]
        run(gens)


D_FF = 2816
D_FFE = 3584
NEXP = 8


def phase_f1(k, l, ph, xsrc, xkeyfn, tiles, GATE):
    P, ins = k.P, k.ins
    mixT = k.dram("mixT", [1024, T], BF16)
    xs = k.dram("xs", [T, 1024], F32)
    h2T = k.dram("h2T", [1024, T], BF16)
    moe = GATE is not None
    g1 = load_modrep(k, 2, ph)
    gm2 = load_modrep(k, 4, ph)
    sh2 = load_modrep(k, 3, ph)
    wout = P.sb([128, 8, 1024], BF16, stack=ph)
    load_cast(k, ph, wout, ins['w_out'][l], 1024)
    if moe:
        rt = P.sb([128, 8, 8], F32, stack=ph)
        P.dma('sp', rt[:], ins['moe_router'][0].rearrange("(kt p) e -> p kt e", p=128), writes=[rt])
        h32 = P.sb([128, 8, 128], F32, stack=ph)
        lg = P.sb([128, 8], F32, stack=ph)
        top8 = P.sb([128, 8], F32, stack=ph)
        p1, p2 = P.sb([128, 1], F32, stack=ph), P.sb([128, 1], F32, stack=ph)
        gt2 = P.sb([128, 8], F32, stack=ph)
    mx = [P.sb([128, 8, 128], BF16, stack=ph) for _ in range(2)]
    xt = [P.sb([128, 1024], F32, stack=ph) for _ in range(2)]
    tmp = P.sb([128, 1024], F32, stack=ph)
    xh = [P.sb([128, 1024], F32, stack=ph) for _ in range(2)]
    junk = P.sb([128, 1024], F32, stack=ph)
    ss = [P.sb([128, 1], F32, stack=ph) for _ in range(2)]
    rstd = [P.sb([128, 1], F32, stack=ph) for _ in range(2)]
    hst = [P.sb([128, 8, 128], BF16, stack=ph) for _ in range(2)]
    mix_keys = [b for kk, b in P.dk.items() if kk[0] == 'mixT']
    def stage1(n, i):
        who = 1 if i < 2 else 0
        m_, x_, h_, s_, r_, o_ = mx[n % 2], xt[n % 2], xh[n % 2], ss[n % 2], rstd[n % 2], hst[n % 2]
        P.dma('sp', m_[:], mixT[:, i * 128:(i + 1) * 128].rearrange("(kt p) t -> p kt t", p=128), reads=mix_keys,
              writes=[m_])
        P.dma('sp', x_[:], xsrc[i * 128:(i + 1) * 128, :], reads=xkeyfn(i), writes=[x_])
        for dh in range(2):
            bk = k.banks[(n % 2) * 2 + dh]
            cs = slice(dh * 512, (dh + 1) * 512)
            for kt in range(8):
                P.op('pe', lambda e, bk=bk, kt=kt, m_=m_, cs=cs: e.matmul(
                    bk[:, :], lhsT=m_[:, kt, :], rhs=wout[:, kt, cs], start=(kt == 0), stop=(kt == 7)),
                    reads=[m_, wout], writes=[bk])
            P.op('dve', lambda e, bk=bk, cs=cs, who=who: e.tensor_tensor(out=tmp[:, cs], in0=bk[:, :], in1=g1[who][:, cs],
                                                                        op=ALU.mult), reads=[bk, g1[who]], writes=[tmp])
        P.op('pool', lambda e, x_=x_: e.tensor_tensor(out=x_[:], in0=x_[:], in1=tmp[:], op=ALU.add),
             reads=[x_, tmp], writes=[x_])
        P.dma('pool', xs[i * 128:(i + 1) * 128, :], x_[:], reads=[x_], writes=[P.dkey('xs', i)])
        rms_rstd(k, x_, junk, s_, r_)
        P.op('dve', lambda e, x_=x_, h_=h_, r_=r_, who=who: e.scalar_tensor_tensor(
            out=h_[:], in0=x_[:], scalar=r_[:, 0:1], in1=gm2[who][:], op0=ALU.mult, op1=ALU.mult),
            reads=[x_, r_, gm2[who]], writes=[h_])
        P.op('pool', lambda e, h_=h_, who=who: e.tensor_tensor(out=h_[:], in0=h_[:], in1=sh2[who][:], op=ALU.add),
             reads=[h_, sh2[who]], writes=[h_])

    def stage2(n, i):
        h_, o_ = xh[n % 2], hst[n % 2]
        for hb in range(2):
            bk = k.banks[4 + (n % 2) * 2 + hb]
            for j in range(4):
                f = hb * 4 + j
                P.op('pe', lambda e, bk=bk, j=j, f=f, h_=h_: e.transpose(
                    out=bk[:, j * 128:(j + 1) * 128], in_=h_[:, f * 128:(f + 1) * 128], identity=k.ident[:]),
                    reads=[h_, k.ident], writes=[bk])
            bv = bk[:, :].rearrange("p (j t) -> p j t", j=4)
            P.op('act', lambda e, bv=bv, hb=hb, o_=o_: e.activation(out=o_[:, hb * 4:(hb + 1) * 4, :], in_=bv,
                                                                    func=AF.Copy), reads=[bk], writes=[o_])
            if moe:
                P.op('dve', lambda e, bv=bv, hb=hb: e.tensor_copy(out=h32[:, hb * 4:(hb + 1) * 4, :], in_=bv),
                     reads=[bk], writes=[h32])
        P.dma('act', h2T[:, i * 128:(i + 1) * 128].rearrange("(kt p) t -> p kt t", p=128), o_[:], reads=[o_],
              writes=[P.dkey('h2T', i)])
        if moe:
            bk = k.banks[(n % 2) * 2]
            for kt in range(8):
                P.op('pe', lambda e, bk=bk, kt=kt: e.matmul(bk[:, 0:8], lhsT=h32[:, kt, :], rhs=rt[:, kt, :],
                                                            start=(kt == 0), stop=(kt == 7)),
                     reads=[h32, rt], writes=[bk])
            P.op('dve', lambda e, bk=bk: e.tensor_copy(out=lg[:], in_=bk[:, 0:8]), reads=[bk], writes=[lg])
            P.op('dve', lambda e: e.max(out=top8[:], in_=lg[:]), reads=[lg], writes=[top8])
            P.op('dve', lambda e: e.tensor_tensor(out=p2[:], in0=top8[:, 1:2], in1=top8[:, 0:1], op=ALU.subtract),
                 reads=[top8], writes=[p2])
            P.op('act', lambda e: e.activation(out=p2[:], in_=p2[:], func=AF.Exp), reads=[p2], writes=[p2])
            P.op('dve', lambda e: e.tensor_scalar(out=p2[:], in0=p2[:], scalar1=1.0, scalar2=None, op0=ALU.add),
                 reads=[p2], writes=[p2])
            P.op('dve', lambda e: e.reciprocal(out=p1[:], in_=p2[:]), reads=[p2], writes=[p1])
            P.op('dve', lambda e: e.tensor_scalar(out=p2[:], in0=p1[:], scalar1=-1.0, scalar2=1.0, op0=ALU.mult,
                                                  op1=ALU.add), reads=[p1], writes=[p2])
            P.op('dve', lambda e, i=i: e.tensor_scalar(out=GATE[:, i, :], in0=lg[:], scalar1=top8[:, 0:1],
                                                       scalar2=p1[:, 0:1], op0=ALU.is_equal, op1=ALU.mult),
                 reads=[lg, top8, p1], writes=[GATE])
            P.op('dve', lambda e: e.tensor_scalar(out=gt2[:], in0=lg[:], scalar1=top8[:, 1:2], scalar2=p2[:, 0:1],
                                                  op0=ALU.is_equal, op1=ALU.mult), reads=[lg, top8, p2], writes=[gt2])
            P.op('dve', lambda e, i=i: e.tensor_tensor(out=GATE[:, i, :], in0=GATE[:, i, :], in1=gt2[:], op=ALU.add),
                 reads=[GATE, gt2], writes=[GATE])

    stage1(0, tiles[0])
    for n, i in enumerate(tiles):
        if n + 1 < len(tiles):
            stage1(n + 1, tiles[n + 1])
        stage2(n, i)


def phase_f2(k, l, ph, tiles, passes, GATE):
    P, ins = k.P, k.ins
    xs = k.dram("xs", [T, 1024], F32)
    h2T = k.dram("h2T", [1024, T], BF16)
    g2 = load_modrep(k, 5, ph)
    NF = 8
    wg = [P.sb([128, 8, NF * 128], BF16, stack=ph) for _ in range(2)]
    wu = [P.sb([128, 8, NF * 128], BF16, stack=ph) for _ in range(2)]
    wd = [P.sb([128, NF, 1024], BF16, stack=ph) for _ in range(2)]
    hb_ = [P.sb([128, 8, 512], BF16, stack=ph) for _ in range(2)]
    act = [P.sb([128, NF, 512], BF16, stack=ph) for _ in range(2)]
    sil = [P.sb([128, 512], F32, stack=ph) for _ in range(2)]
    xt = [P.sb([128, 1024], F32, stack=ph) for _ in range(3)]
    tmp = [P.sb([128, 512], F32, stack=ph) for _ in range(2)]
    blocks = []
    cur = []
    for i in tiles:
        if cur and (len(cur) == 4 or (cur[-1] < 2) != (i < 2) or i != cur[-1] + 1):
            blocks.append(cur)
            cur = []
        cur.append(i)
    if cur:
        blocks.append(cur)
    bi = 0
    xi = 0
    si = 0
    NS = 4
    stage = [P.sb([128, 1024], F32, stack=ph) for _ in range(NS)]
    sctr = [0]

    def feeder(pi):
        wgs, wus, wds, f0, nf, ex = passes[pi]
        g_, u_, d_ = wg[pi % 2], wu[pi % 2], wd[pi % 2]
        L = []
        for kt in range(8):
            rs = slice(kt * 128, (kt + 1) * 128)
            cs = slice(f0 * 128, (f0 + nf) * 128)
            L.append((g_, g_[:, kt, 0:nf * 128], wgs[rs, cs], nf * 128))
            L.append((u_, u_[:, kt, 0:nf * 128], wus[rs, cs], nf * 128))
        for f in range(nf):
            L.append((d_, d_[:, f, :], wds[(f0 + f) * 128:(f0 + f + 1) * 128, :], 1024))
        LAG = 2
        bufs = {}
        for i in range(len(L) + LAG):
            if i < len(L):
                buf = stage[sctr[0] % NS]
                sctr[0] += 1
                bufs[i] = buf
                P.dma('sp', buf[:, 0:L[i][3]], L[i][2], writes=[buf])
            j = i - LAG
            if j >= 0:
                dstbuf, dst, src, w = L[j]
                buf = bufs[j]
                if j % 2 == 0:
                    P.op('dve', lambda e, dst=dst, buf=buf, w=w: e.tensor_copy(out=dst, in_=buf[:, 0:w]),
                         reads=[buf], writes=[dstbuf])
                else:
                    P.op('act', lambda e, dst=dst, buf=buf, w=w: e.activation(out=dst, in_=buf[:, 0:w], func=AF.Copy),
                         reads=[buf], writes=[dstbuf])
            yield

    for _ in feeder(0):
        pass
    items = [blk for _ in passes for blk in blocks]

    def load_h(idx):
        if idx >= len(items):
            return
        blk = items[idx]
        n = len(blk) * 128
        t0 = blk[0] * 128
        h_ = hb_[idx % 2]
        P.dma('sp', h_[:, :, 0:n], h2T[:, t0:t0 + n].rearrange("(kt p) t -> p kt t", p=128),
              reads=[P.dkey('h2T', i) for i in blk], writes=[h_])

    load_h(0)
    for pi, (wgs, wus, wds, f0, nf, ex) in enumerate(passes):
        g_, u_, d_ = wg[pi % 2], wu[pi % 2], wd[pi % 2]
        feed = feeder(pi + 1) if pi + 1 < len(passes) else iter(())
        for blk in blocks:
            n = len(blk) * 128
            t0 = blk[0] * 128
            who = 1 if blk[0] < 2 else 0
            h_, a_ = hb_[bi % 2], act[bi % 2]
            bi += 1
            load_h(bi)
            for f in range(nf):
                next(feed, None)
                ba, bb = k.banks[(f % 2) * 2], k.banks[(f % 2) * 2 + 1]
                s_ = sil[si % 2]
                si += 1
                for (bk, w_) in ((ba, g_), (bb, u_)):
                    for kt in range(8):
                        P.op('pe', lambda e, bk=bk, w_=w_, kt=kt, f=f, h_=h_, n=n: e.matmul(
                            bk[:, 0:n], lhsT=w_[:, kt, f * 128:(f + 1) * 128], rhs=h_[:, kt, 0:n],
                            start=(kt == 0), stop=(kt == 7)), reads=[w_, h_], writes=[bk])
                P.op('act', lambda e, ba=ba, s_=s_, n=n: e.activation(out=s_[:, 0:n], in_=ba[:, 0:n], func=AF.Silu),
                     reads=[ba], writes=[s_])
                P.op('dve', lambda e, bb=bb, s_=s_, a_=a_, f=f, n=n: e.tensor_tensor(
                    out=a_[:, f, 0:n], in0=bb[:, 0:n], in1=s_[:, 0:n], op=ALU.mult), reads=[bb, s_], writes=[a_])
            for j, i in enumerate(blk):
                next(feed, None)
                x_ = xt[xi % 3]
                xi += 1
                P.dma('sp', x_[:], xs[i * 128:(i + 1) * 128, :], reads=[P.dkey('xs', i)], writes=[x_])
                for dh in range(2):
                    bk = k.banks[4 + (xi % 2) * 2 + dh]
                    cs = slice(dh * 512, (dh + 1) * 512)
                    t_ = tmp[dh]
                    for f in range(nf):
                        P.op('pe', lambda e, bk=bk, a_=a_, f=f, j=j, d_=d_, cs=cs, nf=nf: e.matmul(
                            bk[:, :], lhsT=a_[:, f, j * 128:(j + 1) * 128], rhs=d_[:, f, cs], start=(f == 0),
                            stop=(f == nf - 1)), reads=[a_, d_], writes=[bk])
                    if ex is None:
                        P.op('dve', lambda e, bk=bk, t_=t_, cs=cs, who=who: e.tensor_tensor(
                            out=t_[:], in0=bk[:, :], in1=g2[who][:, cs], op=ALU.mult), reads=[bk, g2[who]], writes=[t_])
                    else:
                        P.op('dve', lambda e, bk=bk, t_=t_, cs=cs, who=who, i=i, ex=ex: e.scalar_tensor_tensor(
                            out=t_[:], in0=bk[:, :], scalar=GATE[:, i, ex:ex + 1], in1=g2[who][:, cs], op0=ALU.mult,
                            op1=ALU.mult), reads=[bk, g2[who], GATE], writes=[t_])
                    P.op('pool', lambda e, x_=x_, t_=t_, cs=cs: e.tensor_tensor(out=x_[:, cs], in0=x_[:, cs], in1=t_[:],
                                                                             op=ALU.add), reads=[x_, t_], writes=[x_])
                P.dma('pool', xs[i * 128:(i + 1) * 128, :], x_[:], reads=[x_], writes=[P.dkey('xs', i)])
        for _ in feed:
            pass


def dense_passes(ins):
    g, u, d = ins['ffn_w_gate'][0], ins['ffn_w_up'][0], ins['ffn_w_down'][0]
    return [(g, u, d, 0, 8, None), (g, u, d, 8, 8, None), (g, u, d, 16, 6, None)]


def moe_passes(ins, experts=range(NEXP)):
    ps = []
    for ex in experts:
        g, u, d = ins['moe_w_gate'][0][ex], ins['moe_w_up'][0][ex], ins['moe_w_down'][0][ex]
        for q in range(4):
            ps.append((g, u, d, q * 7, 7, ex))
    return ps


def phase_final(k, ph, out):
    P, ins = k.P, k.ins
    xs = k.dram("xs", [T, 1024], F32)
    fg = P.sb([128, 1024], F32, stack=ph)
    P.dma('sp', fg[:], ins['final_g'].partition_broadcast(128), writes=[fg])
    xt = [P.sb([128, 1024], F32, stack=ph) for _ in range(2)]
    yo = [P.sb([128, 1024], F32, stack=ph) for _ in range(2)]
    junk = P.sb([128, 1024], F32, stack=ph)
    ss = [P.sb([128, 1], F32, stack=ph) for _ in range(2)]
    rstd = [P.sb([128, 1], F32, stack=ph) for _ in range(2)]
    for n, i in enumerate(range(2, NT)):
        x_, y_, s_, r_ = xt[n % 2], yo[n % 2], ss[n % 2], rstd[n % 2]
        P.dma('sp', x_[:], xs[i * 128:(i + 1) * 128, :], reads=[P.dkey('xs', i)], writes=[x_])
        rms_rstd(k, x_, junk, s_, r_)
        P.op('dve', lambda e, x_=x_, y_=y_, r_=r_: e.scalar_tensor_tensor(
            out=y_[:], in0=x_[:], scalar=r_[:, 0:1], in1=fg[:], op0=ALU.mult, op1=ALU.mult),
            reads=[x_, r_, fg], writes=[y_])
        P.dma('sp', out[(i - 2) * 128:(i - 1) * 128, :], y_[:], reads=[y_], writes=[P.dkey('out', i)])


IN_SPECS = {
    'xin': ([T, 1024], F32), 'cT': ([128, 8], F32), 'cctxT': ([128, 8], F32),
    'mod_w': ([2, 1024, 6144], F32), 'mod_b': ([2, 6144], F32), 'norm1_g': ([2, 1024], F32), 'norm2_g': ([2, 1024], F32),
    'w_in': ([2, 1024, 3328], F32), 'w_out': ([2, 1024, 1024], F32),
    'rw_muT': ([2, 128, 10, 2], F32),
    'rw_w0': ([2, 2, 256], F32), 'rw_w2': ([2, 2, 64, 256], F32), 'rw_a0': ([2, 2, 256], F32), 'rw_a2': ([2, 2, 64, 256], F32),
    'rw_g2': ([2, 2, 128, 256], F32), 'rw_k_k': ([2, 256], F32), 'rw_k_a': ([2, 256], F32), 'rw_r_k': ([2, 4, 64], F32),
    'rw_gn_g': ([2, 256], F32), 'rw_gn_b': ([2, 256], F32),
    'cv_dw_wT': ([2, 256, 31], F32), 'cvp': ([2, 128, 2, 3], F32), 'na_btab': ([2, 8, 21, 128, 128], F32),
    'ffn_w_gate': ([1, 1024, D_FF], F32), 'ffn_w_up': ([1, 1024, D_FF], F32), 'ffn_w_down': ([1, D_FF, 1024], F32),
    'moe_router': ([1, 1024, 8], F32), 'moe_w_gate': ([1, 8, 1024, D_FFE], F32), 'moe_w_up': ([1, 8, 1024, D_FFE], F32),
    'moe_w_down': ([1, 8, D_FFE, 1024], F32), 'final_g': ([1024], F32),
}


def host_prep(inp, b):
    m = {'xin': np.ascontiguousarray(np.concatenate([inp['ctx'][b], inp['x'][b]], 0)),
         'cT': np.ascontiguousarray(inp['c'][b].reshape(8, 128).T),
         'cctxT': np.ascontiguousarray(inp['c_ctx'].reshape(8, 128).T)}
    return m


def host_shared(inp):
    m = {}
    m['cv_dw_wT'] = np.ascontiguousarray(inp['cv_dw_w'].transpose(0, 2, 1))
    cvp = np.stack([inp['cv_dw_b'], inp['cv_ln_g'], inp['cv_ln_b']], -1)
    m['cvp'] = np.ascontiguousarray(cvp.reshape(2, 2, 128, 3).transpose(0, 2, 1, 3))
    mu = np.stack([inp['rw_mu_prev'], inp['rw_mu_next']], -1)
    m['rw_muT'] = np.ascontiguousarray(mu.reshape(2, 10, 128, 2).transpose(0, 2, 1, 3))
    m['na_btab'] = np.stack([na_btab_host(inp['na_rpb'][l]) for l in range(2)])
    for n in IN_SPECS:
        if n not in m and n in inp:
            m[n] = np.ascontiguousarray(inp[n], dtype=np.float32)
    return m


def build_full(dbg=(), layers=(0, 1), moe_experts=range(NEXP), stop_after=None):
    nc = bass.Bass("TRN2", target_bir_lowering=False)
    ins = {n: nc.dram_tensor(n, s, d, kind="ExternalInput").ap() for n, (s, d) in IN_SPECS.items()}
    out = nc.dram_tensor("out", [NL, 1024], F32, kind="ExternalOutput").ap()
    with contextlib.ExitStack() as st:
        k = K(nc, st, ins, dbg=dbg)
        P = k.P
        xs = k.dram("xs", [T, 1024], F32)
        for l in layers:
            xsrc = ins['xin'] if l == 0 else xs
            xkey = (lambda i: []) if l == 0 else (lambda i: [P.dkey('xs', i)])
            with contextlib.ExitStack() as ph:
                phase_mod(k, l, ph)
                k.barrier()
            with contextlib.ExitStack() as ph:
                HT = P.sb([128, 8, HTW], BF16, stack=ph)
                with contextlib.ExitStack() as ph2:
                    phase_a(k, l, ph2, xsrc, xkey, HT)
                    k.barrier()
                phase_b(k, l, ph, HT)
            with contextlib.ExitStack() as ph:
                phase_c(k, l, ph)
                k.barrier()
            with contextlib.ExitStack() as ph:
                phase_d(k, l, ph, l == 0)
                k.barrier()
            with contextlib.ExitStack() as ph:
                phase_e(k, l, ph, first_tok=(0 if l == 0 else LC))
                k.barrier()
            tiles = list(range(NT)) if l == 0 else list(range(2, NT))
            with contextlib.ExitStack() as ph0:
                GATE = P.sb([128, NT, 8], F32, stack=ph0) if l == 1 else None
                with contextlib.ExitStack() as ph:
                    phase_f1(k, l, ph, xsrc, xkey, tiles, GATE)
                    k.barrier()
                with contextlib.ExitStack() as ph:
                    passes = dense_passes(ins) if l == 0 else moe_passes(ins, moe_experts)
                    phase_f2(k, l, ph, tiles, passes, GATE)
                    k.barrier()
        with contextlib.ExitStack() as ph:
            phase_final(k, ph, out)
            k.barrier()
        P.wait_all('sp', [b for kk, b in P.dk.items() if kk[0] == 'out'])
        P.finalize()
        k.stats = ({e: len(P.q[e]) for e in ENGS}, P.ninc)
    return nc, k


def kernel(**inputs):
    inp = {n: np.asarray(v) for n, v in inputs.items()}
    nc = build_full()[0]
    shared = host_shared(inp)
    in_maps = []
    for b in range(8):
        m = dict(shared)
        m.update(host_prep(inp, b))
        in_maps.append({n: m[n] for n in IN_SPECS})
    res = run_bass_kernel_spmd(nc, in_maps, core_ids=list(range(8)))
    out = np.stack([np.asarray(res.results[b]['out'], dtype=np.float32) for b in range(8)], 0)
    return out
```

```python
import contextlib
import numpy as np
import concourse.bass as bass
import concourse.mybir as mybir
from concourse.bass_utils import run_bass_kernel_spmd

F32 = mybir.dt.float32
BF16 = mybir.dt.bfloat16
I32 = mybir.dt.int32
U32 = mybir.dt.uint32
AF = mybir.ActivationFunctionType
ALU = mybir.AluOpType
AX = mybir.AxisListType

ENGS = ['pe', 'dve', 'act', 'pool', 'sp']
NDS = 6


class Buf:
    def __init__(self, t, name, excl=False):
        self.t = t
        self.name = name
        self.excl = excl
        self.w = []
        self.r = []

    def __getitem__(self, idx):
        return self.t[idx]


class Prog:
    def __init__(self, nc, stack):
        self.nc = nc
        self.stack = stack
        self.q = {e: [] for e in ENGS}
        self.sem = {e: stack.enter_context(nc.semaphore("s_" + e)) for e in ENGS}
        self.dsem = {qe: [stack.enter_context(nc.semaphore("d_%s_%d" % (qe, i))) for i in range(NDS)]
                     for qe in ['sp', 'act', 'pool']}
        self.dcnt = {qe: [0] * NDS for qe in ['sp', 'act', 'pool']}
        self.drr = {qe: 0 for qe in ['sp', 'act', 'pool']}
        self.dk = {}
        self.nbuf = 0

    def sb(self, shape, dt=F32, name=None, stack=None):
        self.nbuf += 1
        name = name or "sb%d" % self.nbuf
        t = (stack or self.stack).enter_context(self.nc.sbuf_tensor(name, list(shape), dt))
        return Buf(t, name)

    def ps(self, shape, dt=F32, name=None):
        self.nbuf += 1
        name = name or "ps%d" % self.nbuf
        t = self.stack.enter_context(self.nc.psum_tensor(name, list(shape), dt))
        return Buf(t, name, excl=True)

    def dkey(self, *key):
        if key not in self.dk:
            self.dk[key] = Buf(None, str(key))
        return self.dk[key]

    def _deps(self, reads, writes, eng=None):
        deps = []
        for b in reads:
            deps.extend(b.w)
            if b.excl:
                deps.extend(d for d in b.r if not (d[0] == 'e' and d[1] == eng))
        for b in writes:
            deps.extend(b.w)
            deps.extend(b.r)
        return deps

    def _commit(self, ev, reads, writes):
        for b in reads:
            b.r.append(ev)
        for b in writes:
            b.w = [ev]
            b.r = []

    def op(self, eng, fn, reads=(), writes=()):
        deps = self._deps(reads, writes, eng)
        idx = len(self.q[eng])
        ev = ('e', eng, idx)
        self.q[eng].append([deps, fn, 'op', None])
        self._commit(ev, reads, writes)
        return ev

    def dma(self, qe, out, in_, reads=(), writes=(), **kw):
        deps = self._deps(reads, writes)
        k = self.drr[qe]
        self.drr[qe] = (k + 1) % NDS
        prev = self.dcnt[qe][k]
        if prev > 0:
            deps.append(('d', qe, k, prev))
        self.dcnt[qe][k] = prev + 16
        ev = ('d', qe, k, prev + 16)
        self.q[qe].append([deps, (out, in_, kw), 'dma', (qe, k)])
        self._commit(ev, reads, writes)
        return ev

    def finalize(self):
        nc = self.nc
        waited = {e: set() for e in ENGS}
        for e in ENGS:
            for deps, fn, kind, x in self.q[e]:
                for d in deps:
                    if d[0] == 'e':
                        if d[1] == 'pe' and e == 'pe':
                            continue
                        waited[d[1]].add(d[2])
        rank = {}
        for e in ENGS:
            s = sorted(waited[e])
            rank[e] = {i: k + 1 for k, i in enumerate(s)}
        self.ninc = {e: len(waited[e]) for e in ENGS}
        eobj = {'pe': nc.tensor, 'dve': nc.vector, 'act': nc.scalar, 'pool': nc.gpsimd, 'sp': nc.sync}

        def replay(e, engine):
            seen = {}
            for idx, (deps, fn, kind, x) in enumerate(self.q[e]):
                need = {}
                for d in deps:
                    if d[0] == 'e':
                        if d[1] == 'pe' and e == 'pe':
                            continue
                        key = ('e', d[1])
                        val = rank[d[1]][d[2]]
                    else:
                        key = ('d', d[1], d[2])
                        val = d[3]
                    if seen.get(key, 0) >= val:
                        continue
                    if need.get(key, 0) < val:
                        need[key] = val
                for key, val in need.items():
                    seen[key] = val
                    sem = self.sem[key[1]] if key[0] == 'e' else self.dsem[key[1]][key[2]]
                    engine.wait_ge(sem, val)
                if kind == 'op':
                    ins = fn(engine)
                    if idx in rank[e]:
                        ins.then_inc(self.sem[e], 1)
                else:
                    out, in_, kw = fn
                    qe, k = x
                    engine.dma_start(out=out, in_=in_, **kw).then_inc(self.dsem[qe][k], 16)

        with nc.Block() as block:
            @block.tensor
            def _(eng):
                replay('pe', eng)

            @block.vector
            def _(eng):
                replay('dve', eng)

            @block.scalar
            def _(eng):
                replay('act', eng)

            @block.gpsimd
            def _(eng):
                replay('pool', eng)

            @block.sync
            def _(eng):
                replay('sp', eng)

    def wait_all(self, eng, bufs):
        deps = []
        for b in bufs:
            deps.extend(b.w)
        self.q[eng].append([deps, lambda e: e.nop(), 'op', None])


D = 1024
LC = 256
NL = 4096
T = LC + NL
NT = T // 128
HTW = T + 4
INW = 3328
RW_IN = 1280
EPS_RMS = 1e-6


def htcol(t):
    return 1 + t if t < LC else 3 + t


class K:
    def __init__(self, nc, stack, ins, dbg=()):
        self.nc = nc
        self.dbg = set(dbg)
        self.P = Prog(nc, stack)
        self.ins = ins
        self.banks = [self.P.ps([128, 512], F32, name="bank%d" % i) for i in range(8)]
        self.dr = {}
        P = self.P
        self.eps_rms = P.sb([128, 1], F32, name="eps_rms")
        P.op('pool', lambda e: e.memset(self.eps_rms[:], EPS_RMS), writes=[self.eps_rms])
        self.ident = P.sb([128, 128], F32, name="ident")
        P.op('pool', lambda e: e.memset(self.ident[:], 1.0), writes=[self.ident])
        P.op('pool', lambda e: e.affine_select(out=self.ident[:], in_=self.ident[:], pattern=[[-1, 128]],
                                               compare_op=ALU.is_equal, fill=0.0, base=0, channel_multiplier=1),
             reads=[self.ident], writes=[self.ident])

    def dram(self, name, shape, dt):
        if name not in self.dr:
            kind = "ExternalOutput" if name in self.dbg else "Internal"
            self.dr[name] = self.nc.dram_tensor(name, list(shape), dt, kind=kind).ap()
        return self.dr[name]

    def barrier(self):
        P = self.P
        evs = []
        for e in ['pe', 'dve', 'act', 'pool']:
            for idx in range(len(P.q[e]) - 1, -1, -1):
                if P.q[e][idx][2] == 'op' and P.q[e][idx][3] != 'nop':
                    evs.append(('e', e, idx))
                    break
        for qe in ['sp', 'act', 'pool']:
            for k in range(NDS):
                if P.dcnt[qe][k] > 0:
                    evs.append(('d', qe, k, P.dcnt[qe][k]))
        for e in ENGS:
            P.q[e].append([list(evs), (lambda en: en.nop()), 'op', 'nop'])


def phase_mod(k, l, ph):
    P, nc, ins = k.P, k.nc, k.ins
    modrep = k.dram("modrep", [2, 6, 128, 1024], F32)
    cT = P.sb([128, 2, 8], F32, stack=ph)
    P.dma('sp', cT[:, 0, :], ins['cT'], writes=[cT])
    P.dma('sp', cT[:, 1, :], ins['cctxT'], writes=[cT])
    cs = P.sb([128, 2, 8], F32, stack=ph)
    P.op('act', lambda e: e.activation(out=cs[:], in_=cT[:], func=AF.Silu), reads=[cT], writes=[cs])
    csr = P.sb([128, 8, 2, 64], F32, stack=ph)
    P.op('dve', lambda e: e.tensor_copy(out=csr[:], in_=cs[:].rearrange("p w k -> p k w").unsqueeze(3).broadcast_to(
        [128, 8, 2, 64])), reads=[cs], writes=[csr])
    wm = [P.sb([128, 8, 512], F32, stack=ph) for _ in range(2)]
    brep = [P.sb([128, 512], F32, stack=ph) for _ in range(2)]
    grep = [P.sb([128, 512], F32, stack=ph) for _ in range(2)]
    res = [P.sb([128, 512], F32, stack=ph) for _ in range(4)]
    for cc in range(12):
        which, half = cc // 2, cc % 2
        w_, b_, g_ = wm[cc % 2], brep[cc % 2], grep[cc % 2]
        P.dma('sp', w_[:], ins['mod_w'][l][:, cc * 512:(cc + 1) * 512].rearrange("(kt p) n -> p kt n", p=128),
              writes=[w_])
        P.dma('sp', b_[:], ins['mod_b'][l][cc * 512:(cc + 1) * 512].partition_broadcast(128), writes=[b_])
        if which in (1, 4):
            gsrc = ins['norm1_g'] if which == 1 else ins['norm2_g']
            P.dma('sp', g_[:], gsrc[l][half * 512:(half + 1) * 512].partition_broadcast(128), writes=[g_])
        bk = k.banks[cc % 4]
        for kt in range(8):
            P.op('pe', lambda e, kt=kt, w_=w_, bk=bk: e.matmul(
                bk[:, :], lhsT=csr[:, kt, :, :].rearrange("p w j -> p (w j)"), rhs=w_[:, kt, :], start=(kt == 0),
                stop=(kt == 7)), reads=[csr, w_], writes=[bk])
        r_ = res[cc % 4]
        P.op('dve', lambda e, r_=r_, bk=bk, b_=b_: e.tensor_tensor(out=r_[:], in0=bk[:, :], in1=b_[:], op=ALU.add),
             reads=[bk, b_], writes=[r_])
        if which in (1, 4):
            P.op('dve', lambda e, r_=r_, g_=g_: e.scalar_tensor_tensor(
                out=r_[:], in0=r_[:], scalar=1.0, in1=g_[:], op0=ALU.add, op1=ALU.mult),
                reads=[r_, g_], writes=[r_])
        for who in range(2):
            for hp in range(2):
                P.dma('act' if hp else 'sp', modrep[who, which, hp * 64:(hp + 1) * 64, half * 512:(half + 1) * 512],
                      r_[who * 64:(who + 1) * 64, :], reads=[r_], writes=[P.dkey('mod', who, which, half, hp)])


def load_modrep(k, which, ph, eng='sp'):
    P = k.P
    modrep = k.dram("modrep", [2, 6, 128, 1024], F32)
    out = []
    for who in range(2):
        t = P.sb([128, 1024], F32, stack=ph)
        P.dma(eng, t[:], modrep[who, which], reads=[P.dkey('mod', who, which, h_, p_) for h_ in range(2) for p_ in range(2)],
              writes=[t])
        out.append(t)
    return out


def rms_rstd(k, xt, junk, ss, rstd):
    P = k.P
    P.op('act', lambda e: e.activation(out=junk[:], in_=xt[:], func=AF.Square, accum_out=ss[:]),
         reads=[xt], writes=[junk, ss])
    P.op('act', lambda e: e.activation(out=rstd[:], in_=ss[:], func=AF.Ln, scale=1.0 / D, bias=k.eps_rms[:]),
         reads=[ss, k.eps_rms], writes=[rstd])
    P.op('act', lambda e: e.activation(out=rstd[:], in_=rstd[:], func=AF.Exp, scale=-0.5),
         reads=[rstd], writes=[rstd])


def phase_a(k, l, ph, xsrc, xkeyfn, HT):
    P = k.P
    gm = load_modrep(k, 1, ph)
    sh = load_modrep(k, 0, ph)
    for c in (0, LC + 1, LC + 2, HTW - 1):
        P.op('pool', lambda e, c=c: e.memset(HT[:, :, c:c + 1], 0.0), writes=[HT])
    xt = [P.sb([128, 1024], F32, stack=ph) for _ in range(2)]
    xh = [P.sb([128, 1024], F32, stack=ph) for _ in range(2)]
    junk = P.sb([128, 1024], F32, stack=ph)
    ss = [P.sb([128, 1], F32, stack=ph) for _ in range(2)]
    rstd = [P.sb([128, 1], F32, stack=ph) for _ in range(2)]
    def stage1(i):
        who = 1 if i < 2 else 0
        x_, h_, s_, r_ = xt[i % 2], xh[i % 2], ss[i % 2], rstd[i % 2]
        P.dma('sp', x_[:], xsrc[i * 128:(i + 1) * 128, :], reads=xkeyfn(i), writes=[x_])
        rms_rstd(k, x_, junk, s_, r_)
        P.op('dve', lambda e, x_=x_, h_=h_, r_=r_, who=who: e.scalar_tensor_tensor(
            out=h_[:], in0=x_[:], scalar=r_[:, 0:1], in1=gm[who][:], op0=ALU.mult, op1=ALU.mult),
            reads=[x_, r_, gm[who]], writes=[h_])
        P.op('pool', lambda e, h_=h_, who=who: e.tensor_tensor(out=h_[:], in0=h_[:], in1=sh[who][:], op=ALU.add),
             reads=[h_, sh[who]], writes=[h_])

    def stage2(i):
        h_ = xh[i % 2]
        c0 = htcol(i * 128)
        for hb in range(2):
            bk = k.banks[(i % 2) * 2 + hb]
            for j in range(4):
                f = hb * 4 + j
                P.op('pe', lambda e, bk=bk, j=j, f=f, h_=h_: e.transpose(
                    out=bk[:, j * 128:(j + 1) * 128], in_=h_[:, f * 128:(f + 1) * 128], identity=k.ident[:]),
                    reads=[h_, k.ident], writes=[bk])
            if hb == 0:
                P.op('act', lambda e, bk=bk, hb=hb, c0=c0: e.activation(
                    out=HT[:, hb * 4:(hb + 1) * 4, c0:c0 + 128], in_=bk[:, :].rearrange("p (j t) -> p j t", j=4),
                    func=AF.Copy), reads=[bk], writes=[HT])
            else:
                P.op('dve', lambda e, bk=bk, hb=hb, c0=c0: e.tensor_copy(
                    out=HT[:, hb * 4:(hb + 1) * 4, c0:c0 + 128], in_=bk[:, :].rearrange("p (j t) -> p j t", j=4)),
                    reads=[bk], writes=[HT])

    stage1(0)
    for i in range(NT):
        if i + 1 < NT:
            stage1(i + 1)
        stage2(i)


def load_cast(k, g, dst, src2d, ncols):
    P = k.P
    stg = [P.sb([128, ncols], F32, stack=g) for _ in range(3)]
    for kt in range(8):
        b = stg[kt % 3]
        P.dma('sp', b[:], src2d[kt * 128:(kt + 1) * 128, :], writes=[b])
        o = dst[:, kt, :]
        if kt % 2 == 0:
            P.op('dve', lambda e, o=o, b=b: e.tensor_copy(out=o, in_=b[:]), reads=[b], writes=[dst])
        else:
            P.op('act', lambda e, o=o, b=b: e.activation(out=o, in_=b[:], func=AF.Copy), reads=[b], writes=[dst])


def tok_blocks():
    return [(0, LC)] + [(LC + b * 512, 512) for b in range(NL // 512)]


def phase_b(k, l, ph, HT):
    P, ins = k.P, k.ins
    qkT = k.dram("qkT", [1024, T], BF16)
    vtm = k.dram("vtm", [T, 512], BF16)
    utm = k.dram("utm", [T, RW_IN], F32)
    gluT = k.dram("gluT", [256, HTW], BF16)
    w_in = ins['w_in'][l]
    wv3 = lambda c0, c1: w_in[:, c0:c1].rearrange("(kt p) n -> p kt n", p=128)
    with contextlib.ExitStack() as g:
        wqk = P.sb([128, 8, 1024], BF16, stack=g)
        load_cast(k, g, wqk, w_in[:, 0:1024], 1024)
        stg = [P.sb([128, 512], BF16, stack=g) for _ in range(3)]
        n_ = 0
        for (t0, n) in tok_blocks():
            c0 = htcol(t0)
            for ft in range(8):
                bk = k.banks[n_ % 4]
                s_ = stg[n_ % 3]
                n_ += 1
                for kt in range(8):
                    P.op('pe', lambda e, bk=bk, kt=kt, ft=ft, c0=c0, n=n: e.matmul(
                        bk[:, 0:n], lhsT=wqk[:, kt, ft * 128:(ft + 1) * 128], rhs=HT[:, kt, c0:c0 + n],
                        start=(kt == 0), stop=(kt == 7)), reads=[wqk, HT], writes=[bk])
                eng = 'act' if n_ % 2 == 0 else 'dve'
                if eng == 'act':
                    P.op('act', lambda e, bk=bk, s_=s_, n=n: e.activation(out=s_[:, 0:n], in_=bk[:, 0:n], func=AF.Copy),
                         reads=[bk], writes=[s_])
                else:
                    P.op('dve', lambda e, bk=bk, s_=s_, n=n: e.tensor_copy(out=s_[:, 0:n], in_=bk[:, 0:n]),
                         reads=[bk], writes=[s_])
                P.dma('sp', qkT[ft * 128:(ft + 1) * 128, t0:t0 + n], s_[:, 0:n], reads=[s_],
                      writes=[P.dkey('qkT', ft, t0)])
        k.barrier()
    with contextlib.ExitStack() as g:
        wv = P.sb([128, 8, 512], BF16, stack=g)
        load_cast(k, g, wv, w_in[:, 1024:1536], 512)
        stg = [P.sb([128, 512], BF16, stack=g) for _ in range(3)]
        for i in range(NT):
            c0 = htcol(i * 128)
            bk = k.banks[i % 4]
            s_ = stg[i % 3]
            for kt in range(8):
                P.op('pe', lambda e, bk=bk, kt=kt, c0=c0: e.matmul(
                    bk[:, :], lhsT=HT[:, kt, c0:c0 + 128], rhs=wv[:, kt, :], start=(kt == 0), stop=(kt == 7)),
                    reads=[wv, HT], writes=[bk])
            if i % 2 == 0:
                P.op('act', lambda e, bk=bk, s_=s_: e.activation(out=s_[:], in_=bk[:, :], func=AF.Copy),
                     reads=[bk], writes=[s_])
            else:
                P.op('dve', lambda e, bk=bk, s_=s_: e.tensor_copy(out=s_[:], in_=bk[:, :]), reads=[bk], writes=[s_])
            P.dma('sp', vtm[i * 128:(i + 1) * 128, :], s_[:], reads=[s_], writes=[P.dkey('vtm', i)])
        k.barrier()
    with contextlib.ExitStack() as g:
        wrb = P.sb([128, 8, RW_IN], BF16, stack=g)
        load_cast(k, g, wrb, w_in[:, 1536:1536 + RW_IN], RW_IN)
        muT = P.sb([128, 10, 2], F32, stack=g)
        P.dma('sp', muT[:], ins['rw_muT'][l], writes=[muT])
        c0T = P.sb([128, 10, 1], F32, stack=g)
        P.op('dve', lambda e: e.tensor_tensor(out=c0T[:], in0=muT[:, :, 0:1], in1=muT[:, :, 1:2], op=ALU.add),
             reads=[muT], writes=[c0T])
        P.op('dve', lambda e: e.tensor_scalar(out=c0T[:], in0=c0T[:], scalar1=-1.0, scalar2=1.0, op0=ALU.mult,
                                              op1=ALU.add), reads=[c0T], writes=[c0T])
        uT = [P.sb([128, 10, 384], F32, stack=g) for _ in range(2)]
        stg = [P.sb([128, RW_IN], F32, stack=g) for _ in range(2)]
        rblocks = [(0, LC)] + [(LC + 384 * j, 384) for j in range(10)] + [(LC + 3840, 256)]
        tn_ = 0
        for bi, (t0, n) in enumerate(rblocks):
            u_ = uT[bi % 2]
            cc = htcol(t0)
            for ft in range(10):
                bk = k.banks[ft % 4]
                for kt in range(8):
                    P.op('pe', lambda e, bk=bk, kt=kt, ft=ft, cc=cc, n=n: e.matmul(
                        bk[:, 0:n + 2], lhsT=wrb[:, kt, ft * 128:(ft + 1) * 128], rhs=HT[:, kt, cc - 1:cc + n + 1],
                        start=(kt == 0), stop=(kt == 7)), reads=[wrb, HT], writes=[bk])
                P.op('dve', lambda e, bk=bk, u_=u_, ft=ft, n=n: e.tensor_scalar(
                    out=u_[:, ft, 0:n], in0=bk[:, 1:n + 1], scalar1=c0T[:, ft, 0:1], scalar2=None, op0=ALU.mult),
                    reads=[bk, c0T], writes=[u_])
                P.op('dve', lambda e, bk=bk, u_=u_, ft=ft, n=n: e.scalar_tensor_tensor(
                    out=u_[:, ft, 0:n], in0=bk[:, 0:n], scalar=muT[:, ft, 0:1], in1=u_[:, ft, 0:n], op0=ALU.mult,
                    op1=ALU.add), reads=[bk, muT, u_], writes=[u_])
                P.op('dve', lambda e, bk=bk, u_=u_, ft=ft, n=n: e.scalar_tensor_tensor(
                    out=u_[:, ft, 0:n], in0=bk[:, 2:n + 2], scalar=muT[:, ft, 1:2], in1=u_[:, ft, 0:n], op0=ALU.mult,
                    op1=ALU.add), reads=[bk, muT, u_], writes=[u_])
            for j in range(n // 128):
                i = t0 // 128 + j
                s_ = stg[i % 2]
                for (f0, f1) in ((0, 4), (4, 8), (8, 10)):
                    bk = k.banks[4 + tn_ % 4]
                    tn_ += 1
                    for ft in range(f0, f1):
                        P.op('pe', lambda e, bk=bk, ft=ft, f0=f0, u_=u_, j=j: e.transpose(
                            out=bk[:, (ft - f0) * 128:(ft - f0 + 1) * 128], in_=u_[:, ft, j * 128:(j + 1) * 128],
                            identity=k.ident[:]), reads=[u_, k.ident], writes=[bk])
                    w = (f1 - f0) * 128
                    if tn_ % 2 == 0:
                        P.op('act', lambda e, bk=bk, s_=s_, f0=f0, w=w: e.activation(
                            out=s_[:, f0 * 128:f0 * 128 + w], in_=bk[:, 0:w], func=AF.Copy), reads=[bk], writes=[s_])
                    else:
                        P.op('pool' if False else 'dve', lambda e, bk=bk, s_=s_, f0=f0, w=w: e.tensor_copy(
                            out=s_[:, f0 * 128:f0 * 128 + w], in_=bk[:, 0:w]), reads=[bk], writes=[s_])
                P.dma('sp', utm[i * 128:(i + 1) * 128, :], s_[:], reads=[s_], writes=[P.dkey('utm', i)])
        k.barrier()
    with contextlib.ExitStack() as g:
        wc = P.sb([128, 8, 512], BF16, stack=g)
        load_cast(k, g, wc, w_in[:, 1536 + RW_IN:INW], 512)
        sig = [P.sb([128, 512], F32, stack=g) for _ in range(2)]
        stg = [P.sb([128, 512], BF16, stack=g) for _ in range(3)]
        n_ = 0
        for (t0, n) in tok_blocks():
            c0 = htcol(t0)
            for j in range(2):
                ba, bb = k.banks[(n_ % 2) * 2], k.banks[(n_ % 2) * 2 + 1]
                sg, s_ = sig[n_ % 2], stg[n_ % 3]
                n_ += 1
                for (bk, fo) in ((ba, j * 128), (bb, 256 + j * 128)):
                    for kt in range(8):
                        P.op('pe', lambda e, bk=bk, kt=kt, fo=fo, c0=c0, n=n: e.matmul(
                            bk[:, 0:n], lhsT=wc[:, kt, fo:fo + 128], rhs=HT[:, kt, c0:c0 + n],
                            start=(kt == 0), stop=(kt == 7)), reads=[wc, HT], writes=[bk])
                P.op('act', lambda e, bb=bb, sg=sg, n=n: e.activation(out=sg[:, 0:n], in_=bb[:, 0:n], func=AF.Sigmoid),
                     reads=[bb], writes=[sg])
                P.op('dve', lambda e, ba=ba, sg=sg, s_=s_, n=n: e.tensor_tensor(
                    out=s_[:, 0:n], in0=ba[:, 0:n], in1=sg[:, 0:n], op=ALU.mult), reads=[ba, sg], writes=[s_])
                P.dma('sp', gluT[j * 128:(j + 1) * 128, c0:c0 + n], s_[:, 0:n], reads=[s_],
                      writes=[P.dkey('gluT', j, t0)])
        k.barrier()


LN_EPS = 1e-5
GSW = 15 + LC + 30 + NL + 15
SEG = [(0, LC, 15), (LC, NL, 15 + LC + 30)]
NEG = -30000.0


def phase_c(k, l, ph):
    P, ins = k.P, k.ins
    gluT = k.dram("gluT", [256, HTW], BF16)
    mixT = k.dram("mixT", [1024, T], BF16)
    GS = P.sb([128, 2, GSW], BF16, stack=ph)
    P.op('pool', lambda e: e.memset(GS[:], 0.0), writes=[GS])
    for ct in range(2):
        for (t0, n, off) in SEG:
            P.dma('sp', GS[:, ct, off:off + n], gluT[ct * 128:(ct + 1) * 128, htcol(t0):htcol(t0) + n],
                  reads=[b for kk, b in P.dk.items() if kk[0] == 'gluT' and kk[1] == ct], writes=[GS])
    dwT = P.sb([128, 2, 31], F32, stack=ph)
    P.dma('sp', dwT[:], ins['cv_dw_wT'][l].rearrange("(ct p) j -> p ct j", p=128), writes=[dwT])
    cvp = P.sb([128, 2, 3], F32, stack=ph)
    P.dma('sp', cvp[:], ins['cvp'][l], writes=[cvp])
    identb = P.sb([128, 128], BF16, stack=ph)
    P.op('dve', lambda e: e.tensor_copy(out=identb[:], in_=k.ident[:]), reads=[k.ident], writes=[identb])
    DW = P.sb([128, 2, 31, 128], BF16, stack=ph)
    n_ = 0
    for ct in range(2):
        for j in range(31):
            eng = 'dve'
            n_ += 1
            P.op(eng, lambda e, ct=ct, j=j: e.tensor_scalar(out=DW[:, ct, j, :], in0=identb[:],
                                                            scalar1=dwT[:, ct, j:j + 1], scalar2=None, op0=ALU.mult),
                 reads=[identb, dwT], writes=[DW])
    onesM = P.sb([128, 128], F32, stack=ph)
    P.op('pool', lambda e: e.memset(onesM[:], 1.0 / 256), writes=[onesM])
    epsln = P.sb([128, 1], F32, stack=ph)
    P.op('pool', lambda e: e.memset(epsln[:], LN_EPS), writes=[epsln])
    hcs = [P.sb([128, 2, 512], F32, stack=ph) for _ in range(2)]
    sqs = [P.sb([128, 2, 512], F32, stack=ph) for _ in range(2)]
    m2 = P.sb([128, 512], F32, stack=ph)
    var = P.sb([128, 512], F32, stack=ph)
    dd = [P.sb([128, 512], F32, stack=ph) for _ in range(2)]
    stg = [P.sb([128, 512], BF16, stack=ph) for _ in range(3)]
    bi = 0
    si = 0
    for (st0, sn, off) in SEG:
        for t0 in range(st0, st0 + sn, 512):
            n = min(512, st0 + sn - t0)
            h_, q_ = hcs[bi % 2], sqs[bi % 2]
            bi += 1
            bm, bq = k.banks[4], k.banks[5]
            for ct in range(2):
                bk = k.banks[ct]
                base = off + (t0 - st0) - 15
                for j in range(31):
                    P.op('pe', lambda e, bk=bk, ct=ct, j=j, base=base, n=n: e.matmul(
                        bk[:, 0:n], lhsT=DW[:, ct, j, :], rhs=GS[:, ct, base + j:base + j + n],
                        start=(j == 0), stop=(j == 30)), reads=[DW, GS], writes=[bk])
                P.op('act', lambda e, bk=bk, ct=ct, h_=h_, n=n: e.activation(
                    out=h_[:, ct, 0:n], in_=bk[:, 0:n], func=AF.Identity, bias=cvp[:, ct, 0:1]),
                    reads=[bk, cvp], writes=[h_])
                P.op('act', lambda e, bk=bk, ct=ct, q_=q_, n=n: e.activation(
                    out=q_[:, ct, 0:n], in_=bk[:, 0:n], func=AF.Square, bias=cvp[:, ct, 0:1]),
                    reads=[bk, cvp], writes=[q_])
            for ct in range(2):
                P.op('pe', lambda e, ct=ct, h_=h_, n=n: e.matmul(bm[:, 0:n], lhsT=onesM[:], rhs=h_[:, ct, 0:n],
                                                                 start=(ct == 0), stop=(ct == 1)),
                     reads=[onesM, h_], writes=[bm])
            for ct in range(2):
                P.op('pe', lambda e, ct=ct, q_=q_, n=n: e.matmul(bq[:, 0:n], lhsT=onesM[:], rhs=q_[:, ct, 0:n],
                                                                 start=(ct == 0), stop=(ct == 1)),
                     reads=[onesM, q_], writes=[bq])
            P.op('act', lambda e, n=n: e.activation(out=m2[:, 0:n], in_=bm[:, 0:n], func=AF.Square),
                 reads=[bm], writes=[m2])
            P.op('dve', lambda e, n=n: e.tensor_tensor(out=var[:, 0:n], in0=bq[:, 0:n], in1=m2[:, 0:n], op=ALU.subtract),
                 reads=[bq, m2], writes=[var])
            P.op('act', lambda e, n=n: e.activation(out=var[:, 0:n], in_=var[:, 0:n], func=AF.Ln, bias=epsln[:]),
                 reads=[var, epsln], writes=[var])
            P.op('act', lambda e, n=n: e.activation(out=var[:, 0:n], in_=var[:, 0:n], func=AF.Exp, scale=-0.5),
                 reads=[var], writes=[var])
            for ct in range(2):
                d_ = dd[ct]
                s_ = stg[si % 3]
                si += 1
                P.op('dve', lambda e, ct=ct, h_=h_, d_=d_, n=n: e.tensor_tensor(
                    out=d_[:, 0:n], in0=h_[:, ct, 0:n], in1=bm[:, 0:n], op=ALU.subtract),
                    reads=[h_, bm], writes=[d_])
                P.op('dve', lambda e, d_=d_, n=n: e.tensor_tensor(out=d_[:, 0:n], in0=d_[:, 0:n], in1=var[:, 0:n],
                                                                   op=ALU.mult), reads=[d_, var], writes=[d_])
                P.op('act', lambda e, ct=ct, d_=d_, s_=s_, n=n: e.activation(
                    out=s_[:, 0:n], in_=d_[:, 0:n], func=AF.Silu, scale=cvp[:, ct, 1:2], bias=cvp[:, ct, 2:3]),
                    reads=[d_, cvp], writes=[s_])
                P.dma('sp', mixT[768 + ct * 128:768 + (ct + 1) * 128, t0:t0 + n], s_[:, 0:n], reads=[s_],
                      writes=[P.dkey('mixT', 6 + ct, t0)])


def na_cfgs():
    cfgs = [(2, b) for b in range(0, 5)]
    win = {}
    for a in range(32):
        if a in (0, 1, 30, 31):
            blo = 0 if a < 2 else 28
            lst = []
            for b in range(blo, blo + 4):
                lst.append((b, len(cfgs)))
                cfgs.append((a, b))
            win[a] = lst
        else:
            win[a] = [(a + d, d + 2) for d in range(-2, 3)]
    return cfgs, win


def na_btab_host(rpb):
    cfgs, _ = na_cfgs()
    out = np.full((8, len(cfgs), 128, 128), NEG, np.float32)
    p = np.arange(128)
    kr, kc = p // 64, p % 64
    qr, qc = p // 64, p % 64
    for ci, (a, b) in enumerate(cfgs):
        i = 2 * a + qr[None, :]
        krow = 2 * b + kr[:, None]
        rs = np.clip(i - 4, 0, 56)
        vrow = (krow >= rs) & (krow < rs + 8)
        ws = np.clip(qc[None, :] - 8, 0, 48)
        vcol = (kc[:, None] >= ws) & (kc[:, None] < ws + 16)
        ri = np.clip(krow - i + 7, 0, 14)
        cidx = np.clip(kc[:, None] - qc[None, :] + 15, 0, 30)
        valid = vrow & vcol
        for h in range(8):
            g = rpb[h][ri, cidx]
            out[h, ci] = np.where(valid, g, NEG)
    return out


def phase_d(k, l, ph, with_ctx_q):
    P, ins = k.P, k.ins
    qkT = k.dram("qkT", [1024, T], BF16)
    vtm = k.dram("vtm", [T, 512], BF16)
    mixT = k.dram("mixT", [1024, T], BF16)
    cfgs, win = na_cfgs()
    NC_ = len(cfgs)
    QTs = P.sb([128, T], BF16, stack=ph)
    KTm = [P.sb([128, T], BF16, stack=ph) for _ in range(2)]
    Vaug = P.sb([128, NT, 2, 65], BF16, stack=ph)
    BT = P.sb([128, 2, NC_, 128], BF16, stack=ph)
    btstg = [P.sb([128, NC_, 128], F32, stack=ph) for _ in range(2)]
    ident8 = P.sb([128, 128], BF16, stack=ph)
    P.op('dve', lambda e: e.tensor_scalar(out=ident8[:], in0=k.ident[:], scalar1=8.0, scalar2=None, op0=ALU.mult),
         reads=[k.ident], writes=[ident8])
    P.op('pool', lambda e: e.memset(KTm[0][64:128, :], 0.0), writes=[KTm[0]])
    P.op('pool', lambda e: e.memset(KTm[1][0:64, :], 0.0), writes=[KTm[1]])
    P.op('pool', lambda e: e.memset(Vaug[:, :, :, 64:65], 1.0), writes=[Vaug])
    PT = [P.sb([128, 7, 128], BF16, stack=ph) for _ in range(2)]
    rs = [P.sb([128, 2], F32, stack=ph) for _ in range(2)]
    On = [P.sb([128, 128], F32, stack=ph) for _ in range(2)]
    stg = [P.sb([128, 512], BF16, stack=ph) for _ in range(2)]
    qk_keys = lambda ft: [b for kk, b in P.dk.items() if kk[0] == 'qkT' and kk[1] == ft]
    v_keys = [b for kk, b in P.dk.items() if kk[0] == 'vtm']
    qtiles = ([0, 1] if with_ctx_q else []) + list(range(2, NT))
    it = 0
    for ft in range(4):
        P.dma('sp', QTs[:], qkT[ft * 128:(ft + 1) * 128, :], reads=qk_keys(ft), writes=[QTs])
        for par in range(2):
            r0 = 512 + ft * 128 + par * 64
            P.dma('sp', KTm[par][par * 64:(par + 1) * 64, :], qkT[r0:r0 + 64, :], reads=qk_keys(4 + ft),
                  writes=[KTm[par]])
        vsrc = vtm.rearrange("(i p) (f hh d) -> p i f hh d", p=128, hh=2, d=64)
        for hh in range(2):
            for i0 in range(0, NT, 9):
                i1 = min(NT, i0 + 9)
                P.dma('sp', Vaug[:, i0:i1, hh, 0:64], vsrc[:, i0:i1, ft, hh, :], reads=v_keys, writes=[Vaug])
            bs_ = btstg[hh]
            P.dma('sp', bs_[:], ins['na_btab'][l][2 * ft + hh].rearrange("c k q -> k c q"), writes=[bs_])
            if hh == 0:
                P.op('dve', lambda e, bs_=bs_, hh=hh: e.tensor_copy(out=BT[:, hh, :, :], in_=bs_[:]), reads=[bs_], writes=[BT])
            else:
                P.op('act', lambda e, bs_=bs_, hh=hh: e.activation(out=BT[:, hh, :, :], in_=bs_[:], func=AF.Copy),
                     reads=[bs_], writes=[BT])
        sgi = [0]
        units = [(qi, qt, hh) for qi, qt in enumerate(qtiles) for hh in range(2)]

        def slots_of(qt):
            wl = [] if qt < 2 else win[qt - 2]
            return [(0, None), (1, None)] + [(2 + b, cfg) for (b, cfg) in wl]

        def s_exp(ui):
            qi, qt, hh = units[ui]
            slots = slots_of(qt)
            ns = len(slots)
            bS = [k.banks[(ui % 2) * 2], k.banks[(ui % 2) * 2 + 1]]
            pt = PT[ui % 2]
            for s_, (kt_, cfg) in enumerate(slots):
                bk = bS[s_ // 4]
                o0 = (s_ % 4) * 128
                P.op('pe', lambda e, bk=bk, o0=o0, hh=hh, kt_=kt_, qt=qt, s_=s_: e.matmul(
                    bk[:, o0:o0 + 128], lhsT=KTm[hh][:, kt_ * 128:(kt_ + 1) * 128],
                    rhs=QTs[:, qt * 128:(qt + 1) * 128], start=(s_ % 4 == 0), stop=True, skip_group_check=True),
                    reads=[KTm[hh], QTs], writes=[bk])
                if cfg is not None:
                    P.op('pe', lambda e, bk=bk, o0=o0, hh=hh, cfg=cfg: e.matmul(
                        bk[:, o0:o0 + 128], lhsT=ident8[:], rhs=BT[:, hh, cfg, :], start=False, stop=True,
                        skip_group_check=True), reads=[ident8, BT], writes=[bk])
            n0 = min(ns, 4)
            P.op('act', lambda e, pt=pt, b0=bS[0], n0=n0: e.activation(
                out=pt[:, 0:n0, :], in_=b0[:, 0:n0 * 128].rearrange("p (s q) -> p s q", q=128), func=AF.Exp,
                scale=0.125), reads=[bS[0]], writes=[pt])
            if ns > 4:
                P.op('act', lambda e, pt=pt, b1=bS[1], ns=ns: e.activation(
                    out=pt[:, 4:ns, :], in_=b1[:, 0:(ns - 4) * 128].rearrange("p (s q) -> p s q", q=128),
                    func=AF.Exp, scale=0.125), reads=[bS[1]], writes=[pt])

        def pv(ui):
            qi, qt, hh = units[ui]
            slots = slots_of(qt)
            pt = PT[ui % 2]
            bO = k.banks[4 + qi % 2]
            for s_, (kt_, cfg) in enumerate(slots):
                P.op('pe', lambda e, bO=bO, hh=hh, pt=pt, s_=s_, kt_=kt_: e.matmul(
                    bO[:, hh * 65:(hh + 1) * 65], lhsT=pt[:, s_, :], rhs=Vaug[:, kt_, hh, :],
                    start=(hh == 0 and s_ == 0), stop=True, skip_group_check=True),
                    reads=[pt, Vaug], writes=[bO])
            if hh == 1:
                fin(qi, qt, bO)

        def fin(qi, qt, bO):
            r_, o_ = rs[qi % 2], On[qi % 2]
            bOv = bO[:, 0:130].rearrange("p (h d) -> p h d", d=65)
            P.op('dve', lambda e, r_=r_, bOv=bOv: e.reciprocal(out=r_[:], in_=bOv[:, :, 64]), reads=[bO], writes=[r_])
            P.op('dve', lambda e, r_=r_, o_=o_, bOv=bOv: e.tensor_tensor(
                out=o_[:].rearrange("p (h d) -> p h d", d=64), in0=bOv[:, :, 0:64],
                in1=r_[:].unsqueeze(2).broadcast_to([128, 2, 64]), op=ALU.mult), reads=[bO, r_], writes=[o_])
            bT = k.banks[6 + qi % 2]
            P.op('pe', lambda e, bT=bT, o_=o_: e.transpose(out=bT[:, 0:128], in_=o_[:], identity=k.ident[:]),
                 reads=[o_, k.ident], writes=[bT])
            st_ = stg[sgi[0] % 2]
            slot = qi % 4
            P.op('act', lambda e, bT=bT, st_=st_, slot=slot: e.activation(
                out=st_[:, slot * 128:(slot + 1) * 128], in_=bT[:, 0:128], func=AF.Copy), reads=[bT], writes=[st_])
            last = (qi == len(qtiles) - 1)
            if slot == 3 or last or qtiles[qi + 1] != qt + 1:
                q0 = qt - slot
                P.dma('sp', mixT[ft * 128:(ft + 1) * 128, q0 * 128:(qt + 1) * 128], st_[:, 0:(slot + 1) * 128],
                      reads=[st_], writes=[P.dkey('mixT', ft, q0)])
                sgi[0] += 1

        s_exp(0)
        for ui in range(len(units)):
            if ui + 1 < len(units):
                s_exp(ui + 1)
            pv(ui)


CH = 128
LEV = CH.bit_length() - 2
NCTX = LC // CH
NCH = T // CH
GN_EPS = 64e-5
DECAY_C = 0.6065306597126334


def phase_e(k, l, ph, first_tok=0, dbg_n=None, dbg_stage=99, dbg_dirs=2):
    P, ins = k.P, k.ins
    utm = k.dram("utm", [T, RW_IN], F32)
    rwo = k.dram("rwo", [2, T, 256], F32)
    mixT = k.dram("mixT", [1024, T], BF16)
    bctr = [0]

    def nb():
        b = k.banks[bctr[0] % 8]
        bctr[0] += 1
        return b

    sb = lambda shape, dt=F32: P.sb(shape, dt, stack=ph)
    rep = lambda src, n=CH: src.partition_broadcast(n)
    W0rep, A0rep = sb([CH, 2, 256]), sb([CH, 2, 256])
    for d in range(2):
        P.dma('sp', W0rep[:, d, :], rep(ins['rw_w0'][l][d]), writes=[W0rep])
        P.dma('sp', A0rep[:, d, :], rep(ins['rw_a0'][l][d]), writes=[A0rep])
    KKrep, KArep, OMKA, RKrep, GNG, GNB = [sb([CH, 256]) for _ in range(6)]
    P.dma('sp', KKrep[:], rep(ins['rw_k_k'][l]), writes=[KKrep])
    P.dma('sp', KArep[:], rep(ins['rw_k_a'][l]), writes=[KArep])
    P.dma('sp', RKrep[:], rep(ins['rw_r_k'][l].rearrange("h d -> (h d)")), writes=[RKrep])
    P.dma('sp', GNG[:], rep(ins['rw_gn_g'][l]), writes=[GNG])
    P.dma('sp', GNB[:], rep(ins['rw_gn_b'][l]), writes=[GNB])
    P.op('dve', lambda e: e.tensor_scalar(out=OMKA[:], in0=KArep[:], scalar1=-1.0, scalar2=1.0, op0=ALU.mult,
                                          op1=ALU.add), reads=[KArep], writes=[OMKA])
    W2p, A2p, G2 = sb([128, 2, 256]), sb([128, 2, 256]), sb([128, 2, 256])
    P.op('pool', lambda e: e.memset(W2p[:], 0.0), writes=[W2p])
    P.op('pool', lambda e: e.memset(A2p[:], 0.0), writes=[A2p])
    for d in range(2):
        P.dma('sp', W2p[0:64, d, :], ins['rw_w2'][l][d], writes=[W2p])
        P.dma('sp', A2p[64:128, d, :], ins['rw_a2'][l][d], writes=[A2p])
        P.dma('sp', G2[:, d, :], ins['rw_g2'][l][d], writes=[G2])
    eps12, epsgn = sb([CH, 1]), sb([CH, 1])
    P.op('pool', lambda e: e.memset(eps12[:], 1e-12), writes=[eps12])
    P.op('pool', lambda e: e.memset(epsgn[:], GN_EPS), writes=[epsgn])
    ones64 = sb([CH, 128])
    P.op('pool', lambda e: e.memset(ones64[:], 1.0), writes=[ones64])
    Mst, Min = [sb([CH, 4, CH]) for _ in range(2)], [sb([CH, 4, CH]) for _ in range(2)]
    MstT = [sb([CH, 4, CH]) for _ in range(2)]
    for d in range(2):
        sgn = 1 if d == 0 else -1
        for (m, op, tr) in ((Mst[d], ALU.is_gt, 1), (Min[d], ALU.is_ge, 1), (MstT[d], ALU.is_gt, -1)):
            P.op('pool', lambda e, m=m: e.memset(m[:], 1.0), writes=[m])
            P.op('pool', lambda e, m=m, op=op, sg=sgn * tr: e.affine_select(
                out=m[:], in_=m[:], pattern=[[0, 4], [sg, CH]], compare_op=op, fill=0.0, base=0,
                channel_multiplier=-sg), reads=[m], writes=[m])
    MD, MDT, MET = Mst, MstT, None
    if CH == 128:
        SB = sb([CH, 4, CH])
        P.op('pool', lambda e: e.memset(SB[:], 0.0), writes=[SB])
        P.op('pool', lambda e: e.memset(SB[0:64, :, 0:64], 1.0), writes=[SB])
        P.op('pool', lambda e: e.memset(SB[64:128, :, 64:128], 1.0), writes=[SB])
        MD, MDT, MET = [sb([CH, 4, CH]) for _ in range(2)], [sb([CH, 4, CH]) for _ in range(2)], [sb([CH, 4, CH]) for _ in range(2)]
        for d in range(2):
            P.op('dve', lambda e, d=d: e.tensor_tensor(out=MD[d][:], in0=Mst[d][:], in1=SB[:], op=ALU.mult),
                 reads=[Mst[d], SB], writes=[MD[d]])
            P.op('dve', lambda e, d=d: e.tensor_tensor(out=MDT[d][:], in0=MstT[d][:], in1=SB[:], op=ALU.mult),
                 reads=[MstT[d], SB], writes=[MDT[d]])
            P.op('dve', lambda e, d=d: e.tensor_tensor(out=MET[d][:], in0=MstT[d][:], in1=MDT[d][:], op=ALU.subtract),
                 reads=[MstT[d], MDT[d]], writes=[MET[d]])
    I64 = sb([CH, 4, CH])
    P.op('dve', lambda e: e.tensor_copy(out=I64[:], in_=k.ident[0:CH, 0:CH].unsqueeze(1).broadcast_to([CH, 4, CH])),
         reads=[k.ident], writes=[I64])
    bmask = sb([128, 2, 2, 64])
    P.op('pool', lambda e: e.memset(bmask[:], 0.0), writes=[bmask])
    P.op('pool', lambda e: e.memset(bmask[0:64, :, 0, :], 1.0), writes=[bmask])
    P.op('pool', lambda e: e.memset(bmask[64:128, :, 1, :], 1.0), writes=[bmask])
    def alloc_work():
        Uc = [sb([CH, RW_IN]) for _ in range(2)]
        LT = sb([128, 2, CH])
        tmpA, tmpB = sb([CH, 256]), sb([CH, 256])
        LW, Aa, Gs = sb([CH, 256]), sb([CH, 256]), [sb([CH, 256]) for _ in range(2)]
        kkr, sq, kk, zz = sb([CH, 256]), sb([CH, 256]), sb([CH, 256]), sb([CH, 256])
        s4, rn = sb([CH, 4]), sb([CH, 4])
        kd, bb, tt = sb([CH, 256]), sb([CH, 256]), sb([CH, 256])
        EC, EZ, EN, ET, EH = [sb([CH, 256]) for _ in range(5)]
        RT, ZT, BN, KN = [sb([CH, 256]) for _ in range(4)]
        BH, KH, Vb = ([sb([CH, 256], BF16) for _ in range(2)] for _ in range(3))
        PL1, PL2 = [sb([128, 4, CH], BF16) for _ in range(2)], sb([128, 4, CH], BF16)
        EX1 = sb([128, 4, 2, CH], BF16)
        EX2 = sb([128, 4, 2, CH], BF16)
        P.op('pool', lambda e: e.memset(EX1[:], 0.0), writes=[EX1])
        P.op('pool', lambda e: e.memset(EX2[:], 0.0), writes=[EX2])
        Xm, Ym = [sb([CH, 4, CH], BF16) for _ in range(2)], [sb([CH, 4, CH], BF16) for _ in range(2)]
        Tt = [sb([CH, 4, CH], BF16) for _ in range(2)]
        Ttt = [sb([CH, 4, CH], BF16) for _ in range(2)]
        YE, Asb = sb([CH, 4, CH], BF16), sb([CH, 4, CH], BF16)
        Azk, Arb, Ark = ([sb([CH, 4, CH], BF16) for _ in range(2)] for _ in range(3))
        TF = [sb([CH, 4, CH], BF16) for _ in range(2)]
        Wsb, Usb = sb([CH, 256], BF16), sb([CH, 256], BF16)
        Gam = [sb([128, 2, 128]) for _ in range(2)]
        HE = sb([128, 2, 2, 64])
        HEb = sb([128, 2, 2, 64], BF16)
        htmp = sb([128, 2, 2, 64])
        ysq, yn, rk, oo, prev = [sb([CH, 256]) for _ in range(5)]
        bon = [sb([CH, 256]) for _ in range(2)]
        st1, st2, mean, var_ = sb([CH, 4]), sb([CH, 4]), sb([CH, 4]), sb([CH, 4])
        bsum = sb([CH, 4])
        ostg = [sb([128, 2, CH], BF16) for _ in range(2)]

        return dict(Uc=Uc, LT=LT, tmpA=tmpA, tmpB=tmpB, LW=LW, Aa=Aa, Gs=Gs, kkr=kkr, sq=sq, kk=kk, zz=zz, s4=s4, rn=rn, kd=kd, bb=bb, tt=tt, EC=EC, EZ=EZ, EN=EN, ET=ET, EH=EH, RT=RT, ZT=ZT, BN=BN, KN=KN, BH=BH, KH=KH, Vb=Vb, PL1=PL1, PL2=PL2, EX1=EX1, EX2=EX2, Xm=Xm, Ym=Ym, Tt=Tt, Ttt=Ttt, YE=YE, Asb=Asb, Azk=Azk, Arb=Arb, Ark=Ark, TF=TF, Wsb=Wsb, Usb=Usb, Gam=Gam, HE=HE, HEb=HEb, htmp=htmp, ysq=ysq, yn=yn, rk=rk, bon=bon, oo=oo, prev=prev, st1=st1, st2=st2, mean=mean, var_=var_, bsum=bsum, ostg=ostg)

    v4 = lambda t: t[:].rearrange("p (h d) -> p h d", d=64)
    bc4 = lambda t: t[:].unsqueeze(2).broadcast_to([CH, 4, 64])
    TT = lambda eng, out, a, b, op, rd, wr: P.op(eng, lambda e: e.tensor_tensor(out=out, in0=a, in1=b, op=op),
                                                 reads=rd, writes=wr)
    ACT = lambda out, in_, func, rd, wr, **kw: P.op('act', lambda e: e.activation(out=out, in_=in_, func=func, **kw),
                                                    reads=rd, writes=wr)

    ident64 = k.ident[0:CH, 0:CH]

    def TR(bk, out, in_, rd):
        P.op('pe', lambda e: e.transpose(out=out, in_=in_, identity=ident64), reads=list(rd) + [k.ident], writes=[bk])

    def CP(eng, out, in_, rd, wr):
        if eng == 'act':
            P.op('act', lambda e: e.activation(out=out, in_=in_, func=AF.Copy), reads=rd, writes=wr)
        else:
            P.op(eng, lambda e: e.tensor_copy(out=out, in_=in_), reads=rd, writes=wr)

    def TS(eng, out, in0, s1, s2, op0, op1, rd, wr):
        if op1 is None:
            P.op(eng, lambda e: e.tensor_scalar(out=out, in0=in0, scalar1=s1, scalar2=None, op0=op0), reads=rd, writes=wr)
        else:
            P.op(eng, lambda e: e.tensor_scalar(out=out, in0=in0, scalar1=s1, scalar2=s2, op0=op0, op1=op1),
                 reads=rd, writes=wr)

    def RED(out, in_, rd, wr):
        P.op('dve', lambda e: e.tensor_reduce(out=out, in_=in_, axis=AX.X, op=ALU.add), reads=rd, writes=wr)

    def mm(bk, out, lhsT, rhs, start, rd):
        P.op('pe', lambda e: e.matmul(out, lhsT=lhsT, rhs=rhs, start=start, stop=True, skip_group_check=True),
             reads=rd, writes=[bk])

    def pre(d, n, c, W):
        Uc, LT, tmpA, tmpB, LW, Aa, Gs, kkr, sq, kk, zz, s4, rn, kd, bb, tt, EC, EZ, EN, ET, EH, RT, ZT, BN, KN, BH, KH, Vb, PL1, PL2, EX1, EX2, Xm, Ym, Tt, Ttt, YE, Asb, Azk, Arb, Ark, TF, Wsb, Usb, Gam, HE, HEb, htmp, ysq, yn, rk, bon, oo, prev, st1, st2, mean, var_, bsum, ostg = W['Uc'], W['LT'], W['tmpA'], W['tmpB'], W['LW'], W['Aa'], W['Gs'], W['kkr'], W['sq'], W['kk'], W['zz'], W['s4'], W['rn'], W['kd'], W['bb'], W['tt'], W['EC'], W['EZ'], W['EN'], W['ET'], W['EH'], W['RT'], W['ZT'], W['BN'], W['KN'], W['BH'], W['KH'], W['Vb'], W['PL1'], W['PL2'], W['EX1'], W['EX2'], W['Xm'], W['Ym'], W['Tt'], W['Ttt'], W['YE'], W['Asb'], W['Azk'], W['Arb'], W['Ark'], W['TF'], W['Wsb'], W['Usb'], W['Gam'], W['HE'], W['HEb'], W['htmp'], W['ysq'], W['yn'], W['rk'], W['bon'], W['oo'], W['prev'], W['st1'], W['st2'], W['mean'], W['var_'], W['bsum'], W['ostg']
        nb = W["nb_pre"]
        Azk, Arb, Ark, PL1, BH, KH, Vb, Gam, Gs, bon, TF = Azk[n % 2], Arb[n % 2], Ark[n % 2], PL1[n % 2], BH[n % 2], KH[n % 2], Vb[n % 2], Gam[n % 2], Gs[n % 2], bon[n % 2], TF[n % 2]
        pass
        t0 = c * CH
        U_ = Uc[n % 2]
        P.dma('sp', U_[:], utm[t0:t0 + CH, :], reads=[P.dkey('utm', t0 // 128)], writes=[U_])
        r_, kraw, v_ = U_[:, 0:256], U_[:, 256:512], U_[:, 512:768]
        lo = 768 + d * 256
        bL = nb()
        for j in range(2):
            TR(bL, bL[:, j * CH:(j + 1) * CH], U_[:, lo + j * 128:lo + (j + 1) * 128], [U_])
        ACT(LT[0:64, 0, :], bL[0:64, 0:CH], AF.Tanh, [bL], [LT])
        ACT(LT[64:128, 0, :], bL[64:128, 0:CH], AF.Copy, [bL], [LT])
        ACT(LT[:, 1, :], bL[:, CH:2 * CH], AF.Sigmoid, [bL], [LT])
        bX = nb()
        mm(bX, bX[0:CH, 0:256], LT[:, 0, :], W2p[:, d, :], True, [LT, W2p])
        mm(bX, bX[0:CH, 256:512], LT[:, 0, :], A2p[:, d, :], True, [LT, A2p])
        bG = nb()
        mm(bG, bG[0:CH, 0:256], LT[:, 1, :], G2[:, d, :], True, [LT, G2])
        TT('dve', tmpA[:], bX[0:CH, 0:256], W0rep[:, d, :], ALU.add, [bX, W0rep], [tmpA])
        ACT(tmpA[:], tmpA[:], AF.Sigmoid, [tmpA], [tmpA])
        ACT(LW[:], tmpA[:], AF.Copy, [tmpA], [LW], scale=-DECAY_C)
        TT('dve', tmpB[:], bX[0:CH, 256:512], A0rep[:, d, :], ALU.add, [bX, A0rep], [tmpB])
        ACT(Aa[:], tmpB[:], AF.Sigmoid, [tmpB], [Aa])
        ACT(Gs[:], bG[0:CH, 0:256], AF.Copy, [bG], [Gs])
        yield
        TT('dve', kkr[:], kraw, KKrep[:], ALU.mult, [U_, KKrep], [kkr])
        ACT(sq[:], kkr[:], AF.Square, [kkr], [sq])
        RED(s4[:], v4(sq), [sq], [s4])
        ACT(rn[:], s4[:], AF.Ln, [s4, eps12], [rn], bias=eps12[:])
        ACT(rn[:], rn[:], AF.Exp, [rn], [rn], scale=-0.5)
        TT('dve', v4(kk), v4(kkr), bc4(rn), ALU.mult, [kkr, rn], [kk])
        ACT(zz[:], kk[:], AF.Copy, [kk], [zz], scale=-1.0)
        TT('dve', tt[:], Aa[:], KArep[:], ALU.mult, [Aa, KArep], [tt])
        TT('dve', tt[:], tt[:], OMKA[:], ALU.add, [tt, OMKA], [tt])
        TT('dve', kd[:], kraw, tt[:], ALU.mult, [U_, tt], [kd])
        TT('dve', bb[:], kk[:], Aa[:], ALU.mult, [kk, Aa], [bb])
        yield
        bC = nb()
        mm(bC, bC[0:CH, 0:256], Min[d][:, 0, :], LW[:], True, [Min[d], LW])
        mm(bC, bC[0:CH, 256:512], ones64[:, 0:CH], LW[:], True, [ones64, LW])
        yield
        ACT(EC[:], bC[0:CH, 0:256], AF.Exp, [bC], [EC])
        ACT(EN[:], bC[0:CH, 0:256], AF.Exp, [bC], [EN], scale=-1.0)
        ACT(ET[:], bC[0:CH, 256:512], AF.Exp, [bC], [ET])
        TT('dve', EZ[:], bC[0:CH, 0:256], LW[:], ALU.subtract, [bC, LW], [EZ])
        ACT(EZ[:], EZ[:], AF.Exp, [EZ], [EZ])
        yield
        TT('dve', EH[:], ET[:], EN[:], ALU.mult, [ET, EN], [EH])
        TT('dve', RT[:], r_, EC[:], ALU.mult, [U_, EC], [RT])
        TT('dve', ZT[:], zz[:], EZ[:], ALU.mult, [zz, EZ], [ZT])
        TT('dve', BN[:], bb[:], EN[:], ALU.mult, [bb, EN], [BN])
        TT('pool', KN[:], kd[:], EN[:], ALU.mult, [kd, EN], [KN])
        yield
        TT('dve', BH[:], bb[:], EH[:], ALU.mult, [bb, EH], [BH])
        yield
        TT('dve', KH[:], kd[:], EH[:], ALU.mult, [kd, EH], [KH])
        yield
        ACT(Vb[:], U_[:, 512:768], AF.Copy, [U_], [Vb])
        yield
        bT1, bT2 = nb(), nb()
        for j, src in enumerate((RT, ZT)):
            for pr in range(2):
                o0 = (j * 2 + pr) * CH
                TR(bT1, bT1[:, o0:o0 + CH], src[:, pr * 128:(pr + 1) * 128], [src])
        for j, src in enumerate((BN, KN)):
            for pr in range(2):
                o0 = (j * 2 + pr) * CH
                TR(bT2, bT2[:, o0:o0 + CH], src[:, pr * 128:(pr + 1) * 128], [src])
        b1v = lambda p0, p1: bT1[p0:p1, 0:4 * CH].rearrange("p (j t) -> p j t", t=CH)
        b2v = lambda p0, p1: bT2[p0:p1, 0:4 * CH].rearrange("p (j t) -> p j t", t=CH)
        yield
        ACT(PL1[:], b1v(0, 128), AF.Copy, [bT1], [PL1])
        CP('act', PL2[:], b2v(0, 128), [bT2], [PL2])
        yield
        ACT(EX1[0:64, :, 0, :], b1v(0, 64), AF.Copy, [bT1], [EX1])
        CP('dve', EX1[64:128, :, 1, :], b1v(64, 128), [bT1], [EX1])
        yield
        ACT(EX2[0:64, :, 0, :], b2v(0, 64), AF.Copy, [bT2], [EX2])
        yield
        CP('act', EX2[64:128, :, 1, :], b2v(64, 128), [bT2], [EX2])
        yield
        ex = lambda E_, j: E_[:, j, :, :].rearrange("p a t -> p (a t)")
        f4 = lambda t: t[:].rearrange("p h t -> p (h t)")
        prods = [(PL2, 0, EX1, 2, [(Xm[0], MD[d])]),
                 (PL1, 2, EX2, 0, [(Ym[0], MDT[d])] + ([(YE, MET[d])] if CH == 128 else [])),
                 (PL2, 2, EX1, 2, [(Azk, Mst[d])]),
                 (PL2, 0, EX1, 0, [(Arb, Min[d])]),
                 (PL2, 2, EX1, 0, [(Ark, Min[d])])]
        for (L_, lo_, E_, eo_, outs) in prods:
            bA = nb()
            for pr in range(2):
                cs = slice(pr * 2 * CH, (pr + 1) * 2 * CH)
                mm(bA, bA[0:CH, cs], L_[:, lo_ + pr, :], ex(E_, eo_ + pr), pr == 0, [L_, E_])
            for (dst, msk) in outs:
                TT('dve', f4(dst), bA[0:CH, 0:4 * CH], f4(msk), ALU.mult, [bA, msk], [dst])
        yield
        if CH == 128:
            TT('pool', f4(Tt[0]), f4(Xm[0]), f4(I64), ALU.add, [Xm[0], I64], [Tt[0]])
            TT('dve', f4(Ttt[0]), f4(Ym[0]), f4(I64), ALU.add, [Ym[0], I64], [Ttt[0]])
            for lev in range(5):
                Xc, Yc, Tc, Uc_ = Xm[lev % 2], Ym[lev % 2], Tt[lev % 2], Ttt[lev % 2]
                Xn, Yn, Tn, Un = Xm[(lev + 1) % 2], Ym[(lev + 1) % 2], Tt[(lev + 1) % 2], Ttt[(lev + 1) % 2]
                bY = nb()
                for h in range(4):
                    mm(bY, bY[0:CH, h * CH:(h + 1) * CH], Xc[:, h, :], Yc[:, h, :], h == 0, [Xc, Yc])
                bXx = nb()
                for h in range(4):
                    mm(bXx, bXx[0:CH, h * CH:(h + 1) * CH], Yc[:, h, :], Xc[:, h, :], h == 0, [Xc, Yc])
                ACT(f4(Xn), bXx[0:CH, 0:4 * CH], AF.Copy, [bXx], [Xn])
                CP('act', f4(Yn), bY[0:CH, 0:4 * CH], [bY], [Yn])
                yield
                bTt = nb()
                for h in range(4):
                    mm(bTt, bTt[0:CH, h * CH:(h + 1) * CH], Yn[:, h, :], Tc[:, h, :], h == 0, [Yn, Tc])
                bTu = nb()
                for h in range(4):
                    mm(bTu, bTu[0:CH, h * CH:(h + 1) * CH], Xn[:, h, :], Uc_[:, h, :], h == 0, [Xn, Uc_])
                TT('dve', f4(Tn), bTt[0:CH, 0:4 * CH], f4(Tc), ALU.add, [bTt, Tc], [Tn])
                TT('dve', f4(Un), bTu[0:CH, 0:4 * CH], f4(Uc_), ALU.add, [bTu, Uc_], [Un])
                yield
            TD, TDt = Tt[1], Ttt[1]
            bA1 = nb()
            for h in range(4):
                mm(bA1, bA1[0:CH, h * CH:(h + 1) * CH], YE[:, h, :], TD[:, h, :], h == 0, [YE, TD])
            ACT(f4(Asb), bA1[0:CH, 0:4 * CH], AF.Copy, [bA1], [Asb])
            bZ = nb()
            for h in range(4):
                mm(bZ, bZ[0:CH, h * CH:(h + 1) * CH], TDt[:, h, :], Asb[:, h, :], h == 0, [TDt, Asb])
            TT('dve', f4(TF), bZ[0:CH, 0:4 * CH], f4(TD), ALU.add, [bZ, TD], [TF])
            yield
        else:
            TT('pool', f4(Tt[0]), f4(Xm[0]), f4(I64), ALU.add, [Xm[0], I64], [Tt[0]])
            for lev in range(LEV):
                Xc, Yc, Tc = Xm[lev % 2], Ym[lev % 2], Tt[lev % 2]
                Xn, Yn, Tn = Xm[(lev + 1) % 2], Ym[(lev + 1) % 2], (Tt[(lev + 1) % 2] if lev < LEV - 1 else TF)
                bY = nb()
                for h in range(4):
                    mm(bY, bY[0:CH, h * CH:(h + 1) * CH], Xc[:, h, :], Yc[:, h, :], h == 0, [Xc, Yc])
                if lev < LEV - 1:
                    bXx = nb()
                    for h in range(4):
                        mm(bXx, bXx[0:CH, h * CH:(h + 1) * CH], Yc[:, h, :], Xc[:, h, :], h == 0, [Xc, Yc])
                    ACT(f4(Xn), bXx[0:CH, 0:4 * CH], AF.Copy, [bXx], [Xn])
                CP('act', f4(Yn), bY[0:CH, 0:4 * CH], [bY], [Yn])
                bTt = nb()
                for h in range(4):
                    mm(bTt, bTt[0:CH, h * CH:(h + 1) * CH], Yn[:, h, :], Tc[:, h, :], h == 0, [Yn, Tc])
                TT('dve', f4(Tn), bTt[0:CH, 0:4 * CH], f4(Tc), ALU.add, [bTt, Tc], [Tn])
                yield
            yield

        bGm = nb()
        for pr in range(2):
            mm(bGm, bGm[:, pr * 128:(pr + 1) * 128], LW[:, pr * 128:(pr + 1) * 128], ones64[:, :], pr == 0,
               [LW, ones64])
        ACT(Gam[:].rearrange("p a b -> p (a b)"), bGm[:, 0:256], AF.Exp, [bGm], [Gam])
        yield
        TT('dve', rk[:], r_, RKrep[:], ALU.mult, [U_, RKrep], [rk])
        TT('dve', rk[:], rk[:], kd[:], ALU.mult, [rk, kd], [rk])
        RED(bsum[:], v4(rk), [rk], [bsum])
        TT('dve', v4(bon), U_[:, 512:768].rearrange("p (h d) -> p h d", d=64), bc4(bsum), ALU.mult,
           [U_, bsum], [bon])
        yield

    def post(d, n, c, W):
        Uc, LT, tmpA, tmpB, LW, Aa, Gs, kkr, sq, kk, zz, s4, rn, kd, bb, tt, EC, EZ, EN, ET, EH, RT, ZT, BN, KN, BH, KH, Vb, PL1, PL2, EX1, EX2, Xm, Ym, Tt, Ttt, YE, Asb, Azk, Arb, Ark, TF, Wsb, Usb, Gam, HE, HEb, htmp, ysq, yn, rk, bon, oo, prev, st1, st2, mean, var_, bsum, ostg = W['Uc'], W['LT'], W['tmpA'], W['tmpB'], W['LW'], W['Aa'], W['Gs'], W['kkr'], W['sq'], W['kk'], W['zz'], W['s4'], W['rn'], W['kd'], W['bb'], W['tt'], W['EC'], W['EZ'], W['EN'], W['ET'], W['EH'], W['RT'], W['ZT'], W['BN'], W['KN'], W['BH'], W['KH'], W['Vb'], W['PL1'], W['PL2'], W['EX1'], W['EX2'], W['Xm'], W['Ym'], W['Tt'], W['Ttt'], W['YE'], W['Asb'], W['Azk'], W['Arb'], W['Ark'], W['TF'], W['Wsb'], W['Usb'], W['Gam'], W['HE'], W['HEb'], W['htmp'], W['ysq'], W['yn'], W['rk'], W['bon'], W['oo'], W['prev'], W['st1'], W['st2'], W['mean'], W['var_'], W['bsum'], W['ostg']
        nb = W["nb_post"]
        Azk, Arb, Ark, PL1, BH, KH, Vb, Gam, Gs, bon, TF = Azk[n % 2], Arb[n % 2], Ark[n % 2], PL1[n % 2], BH[n % 2], KH[n % 2], Vb[n % 2], Gam[n % 2], Gs[n % 2], bon[n % 2], TF[n % 2]
        t0 = c * CH
        Tf = TF
        f4 = lambda t: t[:].rearrange("p h t -> p (h t)")
        he = lambda pr: HEb[:, pr, :, :].rearrange("p a t -> p (a t)")
        bW = nb()
        for h in range(4):
            mm(bW, bW[0:CH, h * 64:(h + 1) * 64], Azk[:, h, :], Vb[:, h * 64:(h + 1) * 64], h == 0, [Azk, Vb])
        for pr in range(2):
            mm(bW, bW[0:CH, pr * 128:(pr + 1) * 128], PL1[:, 2 + pr, :], he(pr), False, [PL1, HEb])
        ACT(Wsb[:], bW[0:CH, 0:256], AF.Copy, [bW], [Wsb])
        yield
        bU = nb()
        for h in range(4):
            mm(bU, bU[0:CH, h * 64:(h + 1) * 64], Tf[:, h, :], Wsb[:, h * 64:(h + 1) * 64], h == 0, [Tf, Wsb])
        CP('act', Usb[:], bU[0:CH, 0:256], [bU], [Usb])
        yield
        bYy = nb()
        for h in range(4):
            hs = slice(h * 64, (h + 1) * 64)
            mm(bYy, bYy[0:CH, hs], Arb[:, h, :], Usb[:, hs], h == 0, [Arb, Usb])
        for h in range(4):
            hs = slice(h * 64, (h + 1) * 64)
            mm(bYy, bYy[0:CH, hs], Ark[:, h, :], Vb[:, hs], False, [Ark, Vb])
        for pr in range(2):
            mm(bYy, bYy[0:CH, pr * 128:(pr + 1) * 128], PL1[:, pr, :], he(pr), False, [PL1, HEb])
        bH = nb()
        for pr in range(2):
            cs = slice(pr * 128, (pr + 1) * 128)
            mm(bH, bH[:, cs], BH[:, cs], Usb[:, cs], pr == 0, [BH, Usb])
        for pr in range(2):
            cs = slice(pr * 128, (pr + 1) * 128)
            mm(bH, bH[:, cs], KH[:, cs], Vb[:, cs], False, [KH, Vb])
        fl = lambda t: t[:].rearrange("p a b c -> p (a b c)")
        TT('dve', fl(htmp), bH[:, 0:256], fl(bmask), ALU.mult, [bH, bmask], [htmp])
        TT('pool', fl(HE), fl(HE), Gam[:].rearrange("p a b -> p (a b)"), ALU.mult, [HE, Gam], [HE])
        TT('dve', fl(HE), fl(HE), fl(htmp), ALU.add, [HE, htmp], [HE])
        ACT(fl(HEb), fl(HE), AF.Copy, [HE], [HEb])
        yield
        if t0 >= first_tok:
            yv = bYy[0:CH, 0:256].rearrange("p (h d) -> p h d", d=64)
            RED(st1[:], yv, [bYy], [st1])
            ACT(ysq[:], bYy[0:CH, 0:256], AF.Square, [bYy], [ysq])
            RED(st2[:], v4(ysq), [ysq], [st2])
            TS('pool', mean[:], st1[:], 1.0 / 64, None, ALU.mult, None, [st1], [mean])
            TT('pool', var_[:], mean[:], mean[:], ALU.mult, [mean], [var_])
            P.op('dve', lambda e: e.scalar_tensor_tensor(out=var_[:], in0=st2[:], scalar=1.0 / 64, in1=var_[:],
                                                         op0=ALU.mult, op1=ALU.subtract),
                 reads=[st2, var_], writes=[var_])
            ACT(var_[:], var_[:], AF.Ln, [var_, epsgn], [var_], bias=epsgn[:])
            ACT(var_[:], var_[:], AF.Exp, [var_], [var_], scale=-0.5)
            TT('dve', v4(yn), yv, bc4(mean), ALU.subtract, [bYy, mean], [yn])
            TT('dve', v4(yn), v4(yn), bc4(var_), ALU.mult, [yn, var_], [yn])
            TT('pool', yn[:], yn[:], GNG[:], ALU.mult, [yn, GNG], [yn])
            TT('pool', yn[:], yn[:], GNB[:], ALU.add, [yn, GNB], [yn])
            TT('pool', oo[:], yn[:], bon[:], ALU.add, [yn, bon], [oo])
            TT('dve', oo[:], oo[:], Gs[:], ALU.mult, [oo, Gs], [oo])
            n_other = (NCTX - 1 - c if c < NCTX else NCH - 1 - c + NCTX) if d == 0 else c
            if n_other > n:
                P.dma('pool', rwo[d, t0:t0 + CH, :], oo[:], reads=[oo], writes=[P.dkey('rwo', d, c)])
            else:
                P.dma('sp', prev[:], rwo[1 - d, t0:t0 + CH, :], reads=[P.dkey('rwo', 1 - d, c)], writes=[prev])
                TT('dve', oo[:], oo[:], prev[:], ALU.add, [oo, prev], [oo])
                bO = nb()
                for ct in range(2):
                    TR(bO, bO[:, ct * CH:(ct + 1) * CH], oo[:, ct * 128:(ct + 1) * 128], [oo])
                og = ostg[n % 2]
                ACT(og[:].rearrange("p a t -> p (a t)"), bO[:, 0:2 * CH], AF.Copy, [bO], [og])
                for ct in range(2):
                    P.dma('pool', mixT[512 + ct * 128:512 + (ct + 1) * 128, t0:t0 + CH], og[:, ct, :], reads=[og],
                          writes=[P.dkey('mixT', 4 + ct, t0)])
        yield

    Ws = [alloc_work(), alloc_work()]
    for d in range(2):
        for ci, nm in enumerate(("nb_pre", "nb_post")):
            ctr = [0]

            def nb_d(base=d * 4 + ci * 2, ctr=ctr):
                b = k.banks[base + ctr[0] % 2]
                ctr[0] += 1
                return b
            Ws[d][nm] = nb_d
        HE_, HEb_ = Ws[d]['HE'], Ws[d]['HEb']
        P.op('pool', lambda e, HE_=HE_: e.memset(HE_[:], 0.0), writes=[HE_])
        P.op('pool', lambda e, HEb_=HEb_: e.memset(HEb_[:], 0.0), writes=[HEb_])
    orders = [list(range(NCH)), list(range(NCTX - 1, -1, -1)) + list(range(NCH - 1, NCTX - 1, -1))]
    nsteps = dbg_n or NCH

    def run(gens):
        while gens:
            for g in list(gens):
                try:
                    next(g)
                except StopIteration:
                    gens.remove(g)

    run([pre(d, 0, orders[d][0], Ws[d]) for d in range(dbg_dirs)])
    for n in range(nsteps):
        gens = [post(d, n, orders[d][n], Ws[d]) for d in range(dbg_dirs)]
        if n + 1 < nsteps:
            gens += [pre(d, n + 1, orders[d][n + 1], Ws[d]) for d in range(dbg_dirs)]
        run(gens)


D_FF = 2816
D_FFE = 3584
NEXP = 8


def phase_f1(k, l, ph, xsrc, xkeyfn, tiles, GATE):
    P, ins = k.P, k.ins
    mixT = k.dram("mixT", [1024, T], BF16)
    xs = k.dram("xs", [T, 1024], F32)
    h2T = k.dram("h2T", [1024, T], BF16)
    moe = GATE is not None
    g1 = load_modrep(k, 2, ph)
    gm2 = load_modrep(k, 4, ph)
    sh2 = load_modrep(k, 3, ph)
    wout = P.sb([128, 8, 1024], BF16, stack=ph)
    load_cast(k, ph, wout, ins['w_out'][l], 1024)
    if moe:
        rt = P.sb([128, 8, 8], F32, stack=ph)
        P.dma('sp', rt[:], ins['moe_router'][0].rearrange("(kt p) e -> p kt e", p=128), writes=[rt])
        h32 = P.sb([128, 8, 128], F32, stack=ph)
        lg = P.sb([128, 8], F32, stack=ph)
        top8 = P.sb([128, 8], F32, stack=ph)
        p1, p2 = P.sb([128, 1], F32, stack=ph), P.sb([128, 1], F32, stack=ph)
        gt2 = P.sb([128, 8], F32, stack=ph)
    mx = [P.sb([128, 8, 128], BF16, stack=ph) for _ in range(2)]
    xt = [P.sb([128, 1024], F32, stack=ph) for _ in range(2)]
    tmp = P.sb([128, 1024], F32, stack=ph)
    xh = [P.sb([128, 1024], F32, stack=ph) for _ in range(2)]
    junk = P.sb([128, 1024], F32, stack=ph)
    ss = [P.sb([128, 1], F32, stack=ph) for _ in range(2)]
    rstd = [P.sb([128, 1], F32, stack=ph) for _ in range(2)]
    hst = [P.sb([128, 8, 128], BF16, stack=ph) for _ in range(2)]
    mix_keys = [b for kk, b in P.dk.items() if kk[0] == 'mixT']
    def stage1(n, i):
        who = 1 if i < 2 else 0
        m_, x_, h_, s_, r_, o_ = mx[n % 2], xt[n % 2], xh[n % 2], ss[n % 2], rstd[n % 2], hst[n % 2]
        P.dma('sp', m_[:], mixT[:, i * 128:(i + 1) * 128].rearrange("(kt p) t -> p kt t", p=128), reads=mix_keys,
              writes=[m_])
        P.dma('sp', x_[:], xsrc[i * 128:(i + 1) * 128, :], reads=xkeyfn(i), writes=[x_])
        for dh in range(2):
            bk = k.banks[(n % 2) * 2 + dh]
            cs = slice(dh * 512, (dh + 1) * 512)
            for kt in range(8):
                P.op('pe', lambda e, bk=bk, kt=kt, m_=m_, cs=cs: e.matmul(
                    bk[:, :], lhsT=m_[:, kt, :], rhs=wout[:, kt, cs], start=(kt == 0), stop=(kt == 7)),
                    reads=[m_, wout], writes=[bk])
            P.op('dve', lambda e, bk=bk, cs=cs, who=who: e.tensor_tensor(out=tmp[:, cs], in0=bk[:, :], in1=g1[who][:, cs],
                                                                        op=ALU.mult), reads=[bk, g1[who]], writes=[tmp])
        P.op('pool', lambda e, x_=x_: e.tensor_tensor(out=x_[:], in0=x_[:], in1=tmp[:], op=ALU.add),
             reads=[x_, tmp], writes=[x_])
        P.dma('pool', xs[i * 128:(i + 1) * 128, :], x_[:], reads=[x_], writes=[P.dkey('xs', i)])
        rms_rstd(k, x_, junk, s_, r_)
        P.op('dve', lambda e, x_=x_, h_=h_, r_=r_, who=who: e.scalar_tensor_tensor(
            out=h_[:], in0=x_[:], scalar=r_[:, 0:1], in1=gm2[who][:], op0=ALU.mult, op1=ALU.mult),
            reads=[x_, r_, gm2[who]], writes=[h_])
        P.op('pool', lambda e, h_=h_, who=who: e.tensor_tensor(out=h_[:], in0=h_[:], in1=sh2[who][:], op=ALU.add),
             reads=[h_, sh2[who]], writes=[h_])

    def stage2(n, i):
        h_, o_ = xh[n % 2], hst[n % 2]
        for hb in range(2):
            bk = k.banks[4 + (n % 2) * 2 + hb]
            for j in range(4):
                f = hb * 4 + j
                P.op('pe', lambda e, bk=bk, j=j, f=f, h_=h_: e.transpose(
                    out=bk[:, j * 128:(j + 1) * 128], in_=h_[:, f * 128:(f + 1) * 128], identity=k.ident[:]),
                    reads=[h_, k.ident], writes=[bk])
            bv = bk[:, :].rearrange("p (j t) -> p j t", j=4)
            P.op('act', lambda e, bv=bv, hb=hb, o_=o_: e.activation(out=o_[:, hb * 4:(hb + 1) * 4, :], in_=bv,
                                                                    func=AF.Copy), reads=[bk], writes=[o_])
            if moe:
                P.op('dve', lambda e, bv=bv, hb=hb: e.tensor_copy(out=h32[:, hb * 4:(hb + 1) * 4, :], in_=bv),
                     reads=[bk], writes=[h32])
        P.dma('act', h2T[:, i * 128:(i + 1) * 128].rearrange("(kt p) t -> p kt t", p=128), o_[:], reads=[o_],
              writes=[P.dkey('h2T', i)])
        if moe:
            bk = k.banks[(n % 2) * 2]
            for kt in range(8):
                P.op('pe', lambda e, bk=bk, kt=kt: e.matmul(bk[:, 0:8], lhsT=h32[:, kt, :], rhs=rt[:, kt, :],
                                                            start=(kt == 0), stop=(kt == 7)),
                     reads=[h32, rt], writes=[bk])
            P.op('dve', lambda e, bk=bk: e.tensor_copy(out=lg[:], in_=bk[:, 0:8]), reads=[bk], writes=[lg])
            P.op('dve', lambda e: e.max(out=top8[:], in_=lg[:]), reads=[lg], writes=[top8])
            P.op('dve', lambda e: e.tensor_tensor(out=p2[:], in0=top8[:, 1:2], in1=top8[:, 0:1], op=ALU.subtract),
                 reads=[top8], writes=[p2])
            P.op('act', lambda e: e.activation(out=p2[:], in_=p2[:], func=AF.Exp), reads=[p2], writes=[p2])
            P.op('dve', lambda e: e.tensor_scalar(out=p2[:], in0=p2[:], scalar1=1.0, scalar2=None, op0=ALU.add),
                 reads=[p2], writes=[p2])
            P.op('dve', lambda e: e.reciprocal(out=p1[:], in_=p2[:]), reads=[p2], writes=[p1])
            P.op('dve', lambda e: e.tensor_scalar(out=p2[:], in0=p1[:], scalar1=-1.0, scalar2=1.0, op0=ALU.mult,
                                                  op1=ALU.add), reads=[p1], writes=[p2])
            P.op('dve', lambda e, i=i: e.tensor_scalar(out=GATE[:, i, :], in0=lg[:], scalar1=top8[:, 0:1],
                                                       scalar2=p1[:, 0:1], op0=ALU.is_equal, op1=ALU.mult),
                 reads=[lg, top8, p1], writes=[GATE])
            P.op('dve', lambda e: e.tensor_scalar(out=gt2[:], in0=lg[:], scalar1=top8[:, 1:2], scalar2=p2[:, 0:1],
                                                  op0=ALU.is_equal, op1=ALU.mult), reads=[lg, top8, p2], writes=[gt2])
            P.op('dve', lambda e, i=i: e.tensor_tensor(out=GATE[:, i, :], in0=GATE[:, i, :], in1=gt2[:], op=ALU.add),
                 reads=[GATE, gt2], writes=[GATE])

    stage1(0, tiles[0])
    for n, i in enumerate(tiles):
        if n + 1 < len(tiles):
            stage1(n + 1, tiles[n + 1])
        stage2(n, i)


def phase_f2(k, l, ph, tiles, passes, GATE):
    P, ins = k.P, k.ins
    xs = k.dram("xs", [T, 1024], F32)
    h2T = k.dram("h2T", [1024, T], BF16)
    g2 = load_modrep(k, 5, ph)
    NF = 8
    wg = [P.sb([128, 8, NF * 128], BF16, stack=ph) for _ in range(2)]
    wu = [P.sb([128, 8, NF * 128], BF16, stack=ph) for _ in range(2)]
    wd = [P.sb([128, NF, 1024], BF16, stack=ph) for _ in range(2)]
    hb_ = [P.sb([128, 8, 512], BF16, stack=ph) for _ in range(2)]
    act = [P.sb([128, NF, 512], BF16, stack=ph) for _ in range(2)]
    sil = [P.sb([128, 512], F32, stack=ph) for _ in range(2)]
    xt = [P.sb([128, 1024], F32, stack=ph) for _ in range(3)]
    tmp = [P.sb([128, 512], F32, stack=ph) for _ in range(2)]
    blocks = []
    cur = []
    for i in tiles:
        if cur and (len(cur) == 4 or (cur[-1] < 2) != (i < 2) or i != cur[-1] + 1):
            blocks.append(cur)
            cur = []
        cur.append(i)
    if cur:
        blocks.append(cur)
    bi = 0
    xi = 0
    si = 0
    NS = 4
    stage = [P.sb([128, 1024], F32, stack=ph) for _ in range(NS)]
    sctr = [0]

    def feeder(pi):
        wgs, wus, wds, f0, nf, ex = passes[pi]
        g_, u_, d_ = wg[pi % 2], wu[pi % 2], wd[pi % 2]
        L = []
        for kt in range(8):
            rs = slice(kt * 128, (kt + 1) * 128)
            cs = slice(f0 * 128, (f0 + nf) * 128)
            L.append((g_, g_[:, kt, 0:nf * 128], wgs[rs, cs], nf * 128))
            L.append((u_, u_[:, kt, 0:nf * 128], wus[rs, cs], nf * 128))
        for f in range(nf):
            L.append((d_, d_[:, f, :], wds[(f0 + f) * 128:(f0 + f + 1) * 128, :], 1024))
        LAG = 2
        bufs = {}
        for i in range(len(L) + LAG):
            if i < len(L):
                buf = stage[sctr[0] % NS]
                sctr[0] += 1
                bufs[i] = buf
                P.dma('sp', buf[:, 0:L[i][3]], L[i][2], writes=[buf])
            j = i - LAG
            if j >= 0:
                dstbuf, dst, src, w = L[j]
                buf = bufs[j]
                if j % 2 == 0:
                    P.op('dve', lambda e, dst=dst, buf=buf, w=w: e.tensor_copy(out=dst, in_=buf[:, 0:w]),
                         reads=[buf], writes=[dstbuf])
                else:
                    P.op('act', lambda e, dst=dst, buf=buf, w=w: e.activation(out=dst, in_=buf[:, 0:w], func=AF.Copy),
                         reads=[buf], writes=[dstbuf])
            yield

    for _ in feeder(0):
        pass
    items = [blk for _ in passes for blk in blocks]

    def load_h(idx):
        if idx >= len(items):
            return
        blk = items[idx]
        n = len(blk) * 128
        t0 = blk[0] * 128
        h_ = hb_[idx % 2]
        P.dma('sp', h_[:, :, 0:n], h2T[:, t0:t0 + n].rearrange("(kt p) t -> p kt t", p=128),
              reads=[P.dkey('h2T', i) for i in blk], writes=[h_])

    load_h(0)
    for pi, (wgs, wus, wds, f0, nf, ex) in enumerate(passes):
        g_, u_, d_ = wg[pi % 2], wu[pi % 2], wd[pi % 2]
        feed = feeder(pi + 1) if pi + 1 < len(passes) else iter(())
        for blk in blocks:
            n = len(blk) * 128
            t0 = blk[0] * 128
            who = 1 if blk[0] < 2 else 0
            h_, a_ = hb_[bi % 2], act[bi % 2]
            bi += 1
            load_h(bi)
            for f in range(nf):
                next(feed, None)
                ba, bb = k.banks[(f % 2) * 2], k.banks[(f % 2) * 2 + 1]
                s_ = sil[si % 2]
                si += 1
                for (bk, w_) in ((ba, g_), (bb, u_)):
                    for kt in range(8):
                        P.op('pe', lambda e, bk=bk, w_=w_, kt=kt, f=f, h_=h_, n=n: e.matmul(
                            bk[:, 0:n], lhsT=w_[:, kt, f * 128:(f + 1) * 128], rhs=h_[:, kt, 0:n],
                            start=(kt == 0), stop=(kt == 7)), reads=[w_, h_], writes=[bk])
                P.op('act', lambda e, ba=ba, s_=s_, n=n: e.activation(out=s_[:, 0:n], in_=ba[:, 0:n], func=AF.Silu),
                     reads=[ba], writes=[s_])
                P.op('dve', lambda e, bb=bb, s_=s_, a_=a_, f=f, n=n: e.tensor_tensor(
                    out=a_[:, f, 0:n], in0=bb[:, 0:n], in1=s_[:, 0:n], op=ALU.mult), reads=[bb, s_], writes=[a_])
            for j, i in enumerate(blk):
                next(feed, None)
                x_ = xt[xi % 3]
                xi += 1
                P.dma('sp', x_[:], xs[i * 128:(i + 1) * 128, :], reads=[P.dkey('xs', i)], writes=[x_])
                for dh in range(2):
                    bk = k.banks[4 + (xi % 2) * 2 + dh]
                    cs = slice(dh * 512, (dh + 1) * 512)
                    t_ = tmp[dh]
                    for f in range(nf):
                        P.op('pe', lambda e, bk=bk, a_=a_, f=f, j=j, d_=d_, cs=cs, nf=nf: e.matmul(
                            bk[:, :], lhsT=a_[:, f, j * 128:(j + 1) * 128], rhs=d_[:, f, cs], start=(f == 0),
                            stop=(f == nf - 1)), reads=[a_, d_], writes=[bk])
                    if ex is None:
                        P.op('dve', lambda e, bk=bk, t_=t_, cs=cs, who=who: e.tensor_tensor(
                            out=t_[:], in0=bk[:, :], in1=g2[who][:, cs], op=ALU.mult), reads=[bk, g2[who]], writes=[t_])
                    else:
                        P.op('dve', lambda e, bk=bk, t_=t_, cs=cs, who=who, i=i, ex=ex: e.scalar_tensor_tensor(
                            out=t_[:], in0=bk[:, :], scalar=GATE[:, i, ex:ex + 1], in1=g2[who][:, cs], op0=ALU.mult,
                            op1=ALU.mult), reads=[bk, g2[who], GATE], writes=[t_])
                    P.op('pool', lambda e, x_=x_, t_=t_, cs=cs: e.tensor_tensor(out=x_[:, cs], in0=x_[:, cs], in1=t_[:],
                                                                             op=ALU.add), reads=[x_, t_], writes=[x_])
                P.dma('pool', xs[i * 128:(i + 1) * 128, :], x_[:], reads=[x_], writes=[P.dkey('xs', i)])
        for _ in feed:
            pass


def dense_passes(ins):
    g, u, d = ins['ffn_w_gate'][0], ins['ffn_w_up'][0], ins['ffn_w_down'][0]
    return [(g, u, d, 0, 8, None), (g, u, d, 8, 8, None), (g, u, d, 16, 6, None)]


def moe_passes(ins, experts=range(NEXP)):
    ps = []
    for ex in experts:
        g, u, d = ins['moe_w_gate'][0][ex], ins['moe_w_up'][0][ex], ins['moe_w_down'][0][ex]
        for q in range(4):
            ps.append((g, u, d, q * 7, 7, ex))
    return ps


def phase_final(k, ph, out):
    P, ins = k.P, k.ins
    xs = k.dram("xs", [T, 1024], F32)
    fg = P.sb([128, 1024], F32, stack=ph)
    P.dma('sp', fg[:], ins['final_g'].partition_broadcast(128), writes=[fg])
    xt = [P.sb([128, 1024], F32, stack=ph) for _ in range(2)]
    yo = [P.sb([128, 1024], F32, stack=ph) for _ in range(2)]
    junk = P.sb([128, 1024], F32, stack=ph)
    ss = [P.sb([128, 1], F32, stack=ph) for _ in range(2)]
    rstd = [P.sb([128, 1], F32, stack=ph) for _ in range(2)]
    for n, i in enumerate(range(2, NT)):
        x_, y_, s_, r_ = xt[n % 2], yo[n % 2], ss[n % 2], rstd[n % 2]
        P.dma('sp', x_[:], xs[i * 128:(i + 1) * 128, :], reads=[P.dkey('xs', i)], writes=[x_])
        rms_rstd(k, x_, junk, s_, r_)
        P.op('dve', lambda e, x_=x_, y_=y_, r_=r_: e.scalar_tensor_tensor(
            out=y_[:], in0=x_[:], scalar=r_[:, 0:1], in1=fg[:], op0=ALU.mult, op1=ALU.mult),
            reads=[x_, r_, fg], writes=[y_])
        P.dma('sp', out[(i - 2) * 128:(i - 1) * 128, :], y_[:], reads=[y_], writes=[P.dkey('out', i)])


IN_SPECS = {
    'xin': ([T, 1024], F32), 'cT': ([128, 8], F32), 'cctxT': ([128, 8], F32),
    'mod_w': ([2, 1024, 6144], F32), 'mod_b': ([2, 6144], F32), 'norm1_g': ([2, 1024], F32), 'norm2_g': ([2, 1024], F32),
    'w_in': ([2, 1024, 3328], F32), 'w_out': ([2, 1024, 1024], F32),
    'rw_muT': ([2, 128, 10, 2], F32),
    'rw_w0': ([2, 2, 256], F32), 'rw_w2': ([2, 2, 64, 256], F32), 'rw_a0': ([2, 2, 256], F32), 'rw_a2': ([2, 2, 64, 256], F32),
    'rw_g2': ([2, 2, 128, 256], F32), 'rw_k_k': ([2, 256], F32), 'rw_k_a': ([2, 256], F32), 'rw_r_k': ([2, 4, 64], F32),
    'rw_gn_g': ([2, 256], F32), 'rw_gn_b': ([2, 256], F32),
    'cv_dw_wT': ([2, 256, 31], F32), 'cvp': ([2, 128, 2, 3], F32), 'na_btab': ([2, 8, 21, 128, 128], F32),
    'ffn_w_gate': ([1, 1024, D_FF], F32), 'ffn_w_up': ([1, 1024, D_FF], F32), 'ffn_w_down': ([1, D_FF, 1024], F32),
    'moe_router': ([1, 1024, 8], F32), 'moe_w_gate': ([1, 8, 1024, D_FFE], F32), 'moe_w_up': ([1, 8, 1024, D_FFE], F32),
    'moe_w_down': ([1, 8, D_FFE, 1024], F32), 'final_g': ([1024], F32),
}


def host_prep(inp, b):
    m = {'xin': np.ascontiguousarray(np.concatenate([inp['ctx'][b], inp['x'][b]], 0)),
         'cT': np.ascontiguousarray(inp['c'][b].reshape(8, 128).T),
         'cctxT': np.ascontiguousarray(inp['c_ctx'].reshape(8, 128).T)}
    return m


def host_shared(inp):
    m = {}
    m['cv_dw_wT'] = np.ascontiguousarray(inp['cv_dw_w'].transpose(0, 2, 1))
    cvp = np.stack([inp['cv_dw_b'], inp['cv_ln_g'], inp['cv_ln_b']], -1)
    m['cvp'] = np.ascontiguousarray(cvp.reshape(2, 2, 128, 3).transpose(0, 2, 1, 3))
    mu = np.stack([inp['rw_mu_prev'], inp['rw_mu_next']], -1)
    m['rw_muT'] = np.ascontiguousarray(mu.reshape(2, 10, 128, 2).transpose(0, 2, 1, 3))
    m['na_btab'] = np.stack([na_btab_host(inp['na_rpb'][l]) for l in range(2)])
    for n in IN_SPECS:
        if n not in m and n in inp:
            m[n] = np.ascontiguousarray(inp[n], dtype=np.float32)
    return m


def build_full(dbg=(), layers=(0, 1), moe_experts=range(NEXP), stop_after=None):
    nc = bass.Bass("TRN2", target_bir_lowering=False)
    ins = {n: nc.dram_tensor(n, s, d, kind="ExternalInput").ap() for n, (s, d) in IN_SPECS.items()}
    out = nc.dram_tensor("out", [NL, 1024], F32, kind="ExternalOutput").ap()
    with contextlib.ExitStack() as st:
        k = K(nc, st, ins, dbg=dbg)
        P = k.P
        xs = k.dram("xs", [T, 1024], F32)
        for l in layers:
            xsrc = ins['xin'] if l == 0 else xs
            xkey = (lambda i: []) if l == 0 else (lambda i: [P.dkey('xs', i)])
            with contextlib.ExitStack() as ph:
                phase_mod(k, l, ph)
                k.barrier()
            with contextlib.ExitStack() as ph:
                HT = P.sb([128, 8, HTW], BF16, stack=ph)
                with contextlib.ExitStack() as ph2:
                    phase_a(k, l, ph2, xsrc, xkey, HT)
                    k.barrier()
                phase_b(k, l, ph, HT)
            with contextlib.ExitStack() as ph:
                phase_c(k, l, ph)
                k.barrier()
            with contextlib.ExitStack() as ph:
                phase_d(k, l, ph, l == 0)
                k.barrier()
            with contextlib.ExitStack() as ph:
                phase_e(k, l, ph, first_tok=(0 if l == 0 else LC))
                k.barrier()
            tiles = list(range(NT)) if l == 0 else list(range(2, NT))
            with contextlib.ExitStack() as ph0:
                GATE = P.sb([128, NT, 8], F32, stack=ph0) if l == 1 else None
                with contextlib.ExitStack() as ph:
                    phase_f1(k, l, ph, xsrc, xkey, tiles, GATE)
                    k.barrier()
                with contextlib.ExitStack() as ph:
                    passes = dense_passes(ins) if l == 0 else moe_passes(ins, moe_experts)
                    phase_f2(k, l, ph, tiles, passes, GATE)
                    k.barrier()
        with contextlib.ExitStack() as ph:
            phase_final(k, ph, out)
            k.barrier()
        P.wait_all('sp', [b for kk, b in P.dk.items() if kk[0] == 'out'])
        P.finalize()
        k.stats = ({e: len(P.q[e]) for e in ENGS}, P.ninc)
    return nc, k


def kernel(**inputs):
    inp = {n: np.asarray(v) for n, v in inputs.items()}
    nc = build_full()[0]
    shared = host_shared(inp)
    in_maps = []
    for b in range(8):
        m = dict(shared)
        m.update(host_prep(inp, b))
        in_maps.append({n: m[n] for n in IN_SPECS})
    res = run_bass_kernel_spmd(nc, in_maps, core_ids=list(range(8)))
    out = np.stack([np.asarray(res.results[b]['out'], dtype=np.float32) for b in range(8)], 0)
    return out
```
